# Optimizing a Trainium2 kernel written in Bass

```python
import math
import jax, jax.numpy as jnp
from jax import lax
import numpy as np

D_MODEL = 1024
BATCH = 4
SEQ = 4096
DEPTH = 1

A_HEADS = 8
A_HEAD_DIM = 64
A_W = A_HEADS * A_HEAD_DIM
IDX_HEADS = 8
IDX_DIM = 64
IDX_SCALE = (IDX_HEADS * IDX_DIM) ** -0.5
TOPK_MAX = 256
SB_HEADS = 8
SB_HEAD_DIM = 64
SB_W = SB_HEADS * SB_HEAD_DIM
Q_BLOCK = 128
N_BUCKETS = 32
MAX_DISTANCE = 128
N_EXPERTS = 32
TOP_K = 4
D_EXPERT = 1024
SWIGLU_LIMIT = 7.0
SWIGLU_ALPHA = 1.702
EXPERT_BLOCK = 128
LN_EPS = 1e-5
DEEPNORM_ALPHA = (2 * DEPTH) ** 0.25
DEEPNORM_BETA = (8 * DEPTH) ** -0.25
SPLIT_SIZES = (A_W, A_W, A_W, IDX_HEADS * IDX_DIM, IDX_DIM, IDX_HEADS, SB_W, SB_W, SB_W, D_MODEL, D_MODEL)
IN_W = sum(SPLIT_SIZES)

kernel_name = "hybrid_dsa_stickbreak_moe_deepnorm"


def layer_norm(x, g, b):
    xf = x.astype(jnp.float32)
    mu = jnp.mean(xf, axis=-1, keepdims=True)
    var = jnp.mean(jnp.square(xf - mu), axis=-1, keepdims=True)
    y = (xf - mu) * lax.rsqrt(var + LN_EPS)
    return (y * g.astype(jnp.float32) + b.astype(jnp.float32)).astype(x.dtype)


def t5_bucket(rel):
    n = jnp.maximum(rel, 0)
    max_exact = N_BUCKETS // 2
    nf = jnp.maximum(n, 1).astype(jnp.float32)
    large = max_exact + (jnp.log(nf / max_exact) / math.log(MAX_DISTANCE / max_exact) * (N_BUCKETS - max_exact)).astype(jnp.int32)
    large = jnp.minimum(large, N_BUCKETS - 1)
    return jnp.where(n < max_exact, n, large)


def dsa_attention(q, k, v, q_idx, k_idx, w_idx, rel_bias):
    B, S, H, Dh = q.shape
    n_sel = min(TOPK_MAX, S // 4)
    key_pos = jnp.arange(S, dtype=jnp.int32)

    def block(i):
        start = i * Q_BLOCK
        qb = lax.dynamic_slice_in_dim(q, start, Q_BLOCK, axis=1)
        qib = lax.dynamic_slice_in_dim(q_idx, start, Q_BLOCK, axis=1)
        wb = lax.dynamic_slice_in_dim(w_idx, start, Q_BLOCK, axis=1)
        t_pos = start + jnp.arange(Q_BLOCK, dtype=jnp.int32)
        head_scores = jax.nn.relu(jnp.einsum('bthd,bsd->bths', qib, k_idx))
        score = jnp.einsum('bth,bths->bts', wb, head_scores).astype(jnp.float32) * IDX_SCALE
        causal = key_pos[None, :] <= t_pos[:, None]
        score = jnp.where(causal[None], score, -jnp.inf)
        _, sel = lax.top_k(score, n_sel)
        k_sel = jax.vmap(lambda kk, ii: kk[ii])(k, sel)
        v_sel = jax.vmap(lambda vv, ii: vv[ii])(v, sel)
        logits = jnp.einsum('bthd,btkhd->bthk', qb, k_sel).astype(jnp.float32) * (Dh ** -0.5)
        bias = rel_bias[t5_bucket(t_pos[None, :, None] - sel)]
        logits = logits + jnp.moveaxis(bias, -1, 2).astype(jnp.float32)
        valid = (sel <= t_pos[None, :, None])[:, :, None, :]
        logits = jnp.where(valid, logits, -jnp.inf)
        p = jax.nn.softmax(logits, axis=-1).astype(v.dtype)
        return jnp.einsum('bthk,btkhd->bthd', p, v_sel)

    out = lax.map(block, jnp.arange(S // Q_BLOCK, dtype=jnp.int32))
    return jnp.moveaxis(out, 0, 1).reshape(B, S, H * Dh)


def stick_breaking(q, k, v):
    B, S, H, Dh = q.shape
    key_pos = jnp.arange(S, dtype=jnp.int32)

    def block(i):
        start = i * Q_BLOCK
        qb = lax.dynamic_slice_in_dim(q, start, Q_BLOCK, axis=1)
        t_pos = start + jnp.arange(Q_BLOCK, dtype=jnp.int32)
        z = jnp.einsum('bthd,bshd->bhts', qb, k).astype(jnp.float32) * (Dh ** -0.5)
        strict = key_pos[None, :] < t_pos[:, None]
        log_1m = jnp.where(strict, jax.nn.log_sigmoid(-z), 0.0)
        after = lax.cumsum(log_1m, axis=3, reverse=True) - log_1m
        w = jnp.where(strict, jnp.exp(jax.nn.log_sigmoid(z) + after), 0.0)
        return jnp.einsum('bhts,bshd->bthd', w.astype(v.dtype), v)

    out = lax.map(block, jnp.arange(S // Q_BLOCK, dtype=jnp.int32))
    return jnp.moveaxis(out, 0, 1).reshape(B, S, H * Dh)


def token_mixer(h, w_in, w_branch_a, w_branch_b, w_out, rel_bias):
    B, S, _ = h.shape
    proj = h @ w_in
    qa, ka, va, qi, ki, wi, qb, kb, vb, ga, gb = jnp.split(proj, np.cumsum(SPLIT_SIZES)[:-1].tolist(), axis=-1)
    qa = qa.reshape(B, S, A_HEADS, A_HEAD_DIM)
    ka = ka.reshape(B, S, A_HEADS, A_HEAD_DIM)
    va = va.reshape(B, S, A_HEADS, A_HEAD_DIM)
    qi = qi.reshape(B, S, IDX_HEADS, IDX_DIM)
    qb = qb.reshape(B, S, SB_HEADS, SB_HEAD_DIM)
    kb = kb.reshape(B, S, SB_HEADS, SB_HEAD_DIM)
    vb = vb.reshape(B, S, SB_HEADS, SB_HEAD_DIM)
    ya = dsa_attention(qa, ka, va, qi, ki, wi, rel_bias)
    yb = stick_breaking(qb, kb, vb)
    merged = jax.nn.sigmoid(ga) * (ya @ w_branch_a) + jax.nn.sigmoid(gb) * (yb @ w_branch_b)
    return merged @ w_out


def moe(h, w_router, b_router, w_gu, b_gu, w_dn, b_dn):
    Bsz, S, D = h.shape
    T = Bsz * S
    xf = h.reshape(T, D)
    logits = (xf @ w_router + b_router).astype(jnp.float32)
    top_v, top_e = lax.top_k(logits, TOP_K)
    gate = jax.nn.softmax(top_v, axis=-1)
    N = T * TOP_K
    flat_e = top_e.reshape(N).astype(jnp.int32)
    flat_tok = jnp.arange(N, dtype=jnp.int32) // TOP_K
    flat_g = gate.reshape(N)
    order = jnp.argsort(flat_e)
    sorted_e = flat_e[order]
    counts = jnp.zeros((N_EXPERTS,), jnp.int32).at[flat_e].add(1)
    padded = (counts + EXPERT_BLOCK - 1) // EXPERT_BLOCK * EXPERT_BLOCK
    offs = jnp.cumsum(counts) - counts
    pends = jnp.cumsum(padded)
    poffs = pends - padded
    r = jnp.arange(N, dtype=jnp.int32)
    dest = poffs[sorted_e] + (r - offs[sorted_e])
    P = N + N_EXPERTS * EXPERT_BLOCK
    nb = P // EXPERT_BLOCK
    row_tok = jnp.zeros((P,), jnp.int32).at[dest].set(flat_tok[order])
    row_g = jnp.zeros((P,), jnp.float32).at[dest].set(flat_g[order])
    blk_start = jnp.arange(nb, dtype=jnp.int32) * EXPERT_BLOCK
    blk_e = jnp.minimum(jnp.searchsorted(pends, blk_start, side='right'), N_EXPERTS - 1).astype(jnp.int32)

    def block_fn(args):
        tok, g, e = args
        xb = xf[tok]
        hgu = xb @ w_gu[e] + b_gu[e]
        a = jnp.minimum(hgu[:, :D_EXPERT], SWIGLU_LIMIT)
        u = jnp.clip(hgu[:, D_EXPERT:], -SWIGLU_LIMIT, SWIGLU_LIMIT)
        glu = a * jax.nn.sigmoid(a * SWIGLU_ALPHA)
        y = ((u + 1.0) * glu) @ w_dn[e] + b_dn[e]
        return y * g[:, None].astype(y.dtype)

    ys = lax.map(block_fn, (row_tok.reshape(nb, EXPERT_BLOCK), row_g.reshape(nb, EXPERT_BLOCK), blk_e))
    out = jnp.zeros((T, D), h.dtype).at[row_tok].add(ys.reshape(P, D).astype(h.dtype))
    return out.reshape(Bsz, S, D)


def setup_inputs(seed: int = 0) -> dict:
    key = jax.random.key(seed)
    ks = jax.random.split(key, 16)
    L = DEPTH
    E = N_EXPERTS
    F = D_EXPERT

    def nrm(k, shape, scale):
        return jax.random.normal(k, shape, jnp.float32) * scale

    return {
        "x": nrm(ks[0], (BATCH, SEQ, D_MODEL), 1.0),
        "w_in": nrm(ks[1], (L, D_MODEL, IN_W), D_MODEL ** -0.5),
        "w_branch_a": nrm(ks[2], (L, A_W, D_MODEL), A_W ** -0.5),
        "w_branch_b": nrm(ks[3], (L, SB_W, D_MODEL), SB_W ** -0.5),
        "w_out": nrm(ks[4], (L, D_MODEL, D_MODEL), D_MODEL ** -0.5 * DEEPNORM_BETA),
        "rel_bias": nrm(ks[5], (N_BUCKETS, A_HEADS), 0.2),
        "ln1_g": 1.0 + nrm(ks[6], (L, D_MODEL), 0.02),
        "ln1_b": nrm(ks[7], (L, D_MODEL), 0.02),
        "w_router": nrm(ks[8], (L, D_MODEL, E), D_MODEL ** -0.5),
        "b_router": nrm(ks[9], (L, E), 0.01),
        "w_gate_up": nrm(ks[10], (L, E, D_MODEL, 2 * F), D_MODEL ** -0.5),
        "b_gate_up": nrm(ks[11], (L, E, 2 * F), 0.01),
        "w_down": nrm(ks[12], (L, E, F, D_MODEL), F ** -0.5 * DEEPNORM_BETA),
        "b_down": nrm(ks[13], (L, E, D_MODEL), 0.01),
        "ln2_g": 1.0 + nrm(ks[14], (L, D_MODEL), 0.02),
        "ln2_b": nrm(ks[15], (L, D_MODEL), 0.02),
    }


def reference(x, w_in, w_branch_a, w_branch_b, w_out, rel_bias, ln1_g, ln1_b, w_router, b_router, w_gate_up, b_gate_up, w_down, b_down, ln2_g, ln2_b):
    h = x
    for l in range(DEPTH):
        m = token_mixer(h, w_in[l], w_branch_a[l], w_branch_b[l], w_out[l], rel_bias)
        h = layer_norm(DEEPNORM_ALPHA * h + m, ln1_g[l], ln1_b[l])
        f = moe(h, w_router[l], b_router[l], w_gate_up[l], b_gate_up[l], w_down[l], b_down[l])
        h = layer_norm(DEEPNORM_ALPHA * h + f, ln2_g[l], ln2_b[l])
    return h
```

```python
import math
from contextlib import ExitStack

import numpy as np
import ml_dtypes

import concourse.bass as bass
import concourse.mybir as mybir
from concourse.bass_utils import run_bass_kernel_spmd

F32 = mybir.dt.float32
BF16 = mybir.dt.bfloat16
U8 = mybir.dt.uint8
ALU = mybir.AluOpType
AF = mybir.ActivationFunctionType
bf16 = ml_dtypes.bfloat16

NCORES = 8
S = 4096
D = 1024
NLG = 4
TQ = 512
NOWN = NLG * TQ
NBLK = NOWN // 128
NE = 32
CAP = 320
CBS = [(0, 128), (128, 128), (256, 64)]
NIT = 14
NEG = -30000.0
ALPHA = 2.0 ** 0.25
LN_EPS = 1e-5
GROUPS = {0: [0, 3, 4, 7], 1: [1, 2, 5, 6]}

ENGS = ("pe", "act", "dve", "pool", "sp")
NDMASEM = 12


class Prog:
    def __init__(self, nc, stack):
        self.nc = nc
        self.ops = []
        self.sems = {e: stack.enter_context(nc.semaphore("s_" + e)) for e in ENGS}
        self.dsems = {q: [stack.enter_context(nc.semaphore("d_%s_%d" % (q, r))) for r in range(NDMASEM)]
                      for q in ("sp", "act", "pool")}
        self.cnt = {e: 0 for e in ENGS}
        self.dcnt = {q: 0 for q in self.dsems}
        self.waited_d = {e: {} for e in ENGS}
        self.nops = 0

    def op(self, eng, fn, reads=(), writes=(), dma=False, big=False):
        self.ops.append(dict(eng=eng, fn=fn, reads=tuple(reads), writes=tuple(writes), dma=dma, big=big))

    def dma(self, q, out, in_, reads=(), writes=()):
        self.op(q, lambda e: e.dma_start(out=out, in_=in_), reads, writes, dma=True)

    def emit(self):
        nc = self.nc
        dkeys = set()
        for o in self.ops:
            if o["dma"]:
                dkeys.update(o["writes"])
        self.op("sp", None, reads=sorted(dkeys, key=str))
        ops = self.ops
        n = len(ops)
        self.nops += n
        last_w, readers = {}, {}
        deps = [None] * n
        raw = [None] * n
        for i, o in enumerate(ops):
            d = set()
            for k in o["reads"]:
                if k in last_w:
                    d.add(last_w[k])
            raw[i] = set(d)
            for k in o["writes"]:
                if k in last_w:
                    d.add(last_w[k])
                for r in readers.get(k, ()):
                    d.add(r)
            d.discard(i)
            deps[i] = d
            for k in o["writes"]:
                last_w[k] = i
                readers[k] = []
            for k in o["reads"]:
                readers.setdefault(k, []).append(i)
        seen = {e: {f: -1 for f in ENGS} for e in ENGS}
        need = [None] * n
        signal = [False] * n
        for i, o in enumerate(ops):
            E = o["eng"]
            best, keep = {}, []
            for j in deps[i]:
                oj = ops[j]
                if oj["dma"]:
                    keep.append(j)
                    continue
                Fe = oj["eng"]
                if Fe == E:
                    if E in ("pe", "sp"):
                        continue
                if seen[E][Fe] >= j:
                    continue
                if Fe not in best or best[Fe] < j:
                    best[Fe] = j
            for Fe, j in best.items():
                keep.append(j)
                seen[E][Fe] = j
                signal[j] = True
            need[i] = sorted(keep)
        need[n - 1] = sorted(set(need[n - 1]) | {i for i in range(n) if ops[i]["dma"]})
        sigval = [0] * n
        dinfo = {}
        acts = {e: [] for e in ENGS}
        for i, o in enumerate(ops):
            E = o["eng"]
            for j in need[i]:
                oj = ops[j]
                if oj["dma"]:
                    s, v, key = dinfo[j]
                    if self.waited_d[E].get(key, 0) >= v:
                        continue
                    self.waited_d[E][key] = v
                    acts[E].append(("w", s, v))
                else:
                    acts[E].append(("w", self.sems[oj["eng"]], sigval[j]))
            if o["dma"]:
                k = self.dcnt[E]
                self.dcnt[E] += 1
                r = k % NDMASEM
                s = self.dsems[E][r]
                if k >= NDMASEM:
                    v0 = 16 * (k // NDMASEM)
                    key = (E, r)
                    if self.waited_d[E].get(key, 0) < v0:
                        acts[E].append(("w", s, v0))
                        self.waited_d[E][key] = v0
                acts[E].append(("o", o["fn"], s, 16))
                dinfo[i] = (s, 16 * (k // NDMASEM + 1), (E, r))
            elif o["fn"] is not None:
                if signal[i]:
                    self.cnt[E] += 1
                    sigval[i] = self.cnt[E]
                    acts[E].append(("o", o["fn"], self.sems[E], 1))
                else:
                    acts[E].append(("o", o["fn"], None, 0))

        def replay(E):
            def f(e):
                for a in acts[E]:
                    if a[0] == "w":
                        e.wait_ge(a[1], a[2])
                    else:
                        inst = a[1](e)
                        if a[2] is not None:
                            inst.then_inc(a[2], a[3])
            return f

        with nc.Block() as block:
            block.tensor(replay("pe"))
            block.scalar(replay("act"))
            block.vector(replay("dve"))
            block.gpsimd(replay("pool"))
            block.sync(replay("sp"))
        nc.all_engine_barrier()
        self.ops = []


class Ring:
    def __init__(self, tiles, name):
        self.tiles = tiles
        self.name = name
        self.i = 0

    def next(self):
        k = self.i % len(self.tiles)
        self.i += 1
        return self.tiles[k], "%s%d" % (self.name, k)


def mm_group(P, out, pairs, reads, wkey, start=True, stop=True):
    def fn(e):
        inst = None
        n = len(pairs)
        for i, p in enumerate(pairs):
            if len(p) == 3:
                o, l, r = p
            else:
                o = out
                l, r = p
            inst = e.matmul(o, lhsT=l, rhs=r, start=(start and i == 0), stop=(stop and i == n - 1),
                            skip_group_check=True)
        return inst
    P.op("pe", fn, reads=reads, writes=[wkey])


def build_program(debug=False):
    nc = bass.Bass("TRN2", target_bir_lowering=False)

    def din(name, shape, dt=F32):
        return nc.dram_tensor(name, list(shape), dt, kind="ExternalInput").ap()

    xT = din("xT", [D, S])
    xTq = din("xTq", [D, NOWN])
    xq = din("xq", [NOWN, D])
    wKA = din("wKA", [D, 1152])
    wKB = din("wKB", [D, 1024])
    wQA = din("wQA", [D, 1024])
    wWI = din("wWI", [D, 8])
    wQB = din("wQB", [D, 512])
    wG = din("wG", [D, 2048])
    wA = din("wA", [512, D])
    wB = din("wB", [512, D])
    wO = din("wO", [D, D])
    wR = din("wR", [D, NE])
    wGU = din("wGU", [NE, D, 2048])
    wDN = din("wDN", [NE, D, D])
    bGU = din("bGU", [128, NE * 16])
    bDN = din("bDN", [NE, D])
    bRT = din("bRT", [128, NE])
    lnp = din("lnp", [4, 128, D])
    rb31 = din("rb31", [128, 8])
    t5tab = din("t5tab", [2, 8, 9, 128, TQ], BF16)
    sbmask = din("sbmask", [2, 8, 128, TQ], BF16)
    cmask = din("cmask", [2, 4, 128, 1024], BF16)
    cst_b = din("cst_b", [128, 5 * 128], BF16)
    cst_f = din("cst_f", [128, 128 + NIT + 1 + CAP])
    out = nc.dram_tensor("out", [NOWN, D], F32, kind="ExternalOutput").ap()
    dk = dict(kind="ExternalOutput") if debug else {}
    ya_d = nc.dram_tensor("ya_d", [512, NOWN], BF16, **dk).ap()
    yb_d = nc.dram_tensor("yb_d", [512, NOWN], BF16, **dk).ap()
    h1_d = nc.dram_tensor("h1_d", [NOWN, D], F32, **dk).ap()
    if debug:
        dbg_f = nc.dram_tensor("dbg_f", [128, NBLK, D], F32, kind="ExternalOutput").ap()
        dbg_g = nc.dram_tensor("dbg_g", [128, NBLK, NE], F32, kind="ExternalOutput").ap()
        dbg_p = nc.dram_tensor("dbg_p", [128, NBLK, NE], F32, kind="ExternalOutput").ap()
        dbg_x = nc.dram_tensor("dbg_x", [128, 8, CAP], BF16, kind="ExternalOutput").ap()
        dbg_a = nc.dram_tensor("dbg_a", [128, 8, CAP], BF16, kind="ExternalOutput").ap()
        dbg_y = nc.dram_tensor("dbg_y", [128, 3, D], BF16, kind="ExternalOutput").ap()

    top = ExitStack()
    with top:
        P = Prog(nc, top)

        def sb(st, name, shape, dt):
            return st.enter_context(nc.sbuf_tensor(name, list(shape), dt))

        def ps(st, name, shape=(128, 512), dt=F32):
            return st.enter_context(nc.psum_tensor(name, list(shape), dt))

        cb = sb(top, "cb", [128, 5 * 128], BF16)
        cf = sb(top, "cf", [128, 128 + NIT + 1 + CAP], F32)
        ident, negU, negones, ustrict, ones = [cb[:, i * 128:(i + 1) * 128] for i in range(5)]
        identf = cf[:, 0:128]
        pow2 = cf[:, 128:128 + NIT + 1]
        iota = cf[:, 128 + NIT + 1:128 + NIT + 1 + CAP]
        gates_all = sb(top, "gates_all", [128, NBLK, NE], F32)
        mask_b = sb(top, "mask_b", [128, NBLK, NE], BF16)
        mask_f = sb(top, "mask_f", [128, NBLK, NE], F32)
        rb31s = sb(top, "rb31s", [128, 8], F32)
        P.dma("sp", cb[:], cst_b[:, :], writes=["cb"])
        P.dma("sp", cf[:], cst_f[:, :], writes=["cf"])
        P.dma("sp", rb31s[:], rb31[:, :], writes=["rb31s"])
        psb = [ps(top, "psb%d" % i) for i in range(8)]

        def kside(st, wsrc, ncols_fm, fm_dst, tm_dst, tag):
            ncol = ncols_fm + 512
            w = sb(st, "wk" + tag, [128, 8, ncol], BF16)
            P.dma("pool", w[:], wsrc.rearrange("(c p) n -> p c n", p=128), writes=["wk"])
            xr = Ring([sb(st, "xk%s%d" % (tag, i), [128, 8, 512], BF16) for i in range(2)], "xk")
            pr = Ring(psb[0:4], "psb")
            pr.i = 0
            xTv = xT.rearrange("(c p) t -> p c t", p=128)
            for tc in range(8):
                xt, xk = xr.next()
                P.dma("pool", xt[:], xTv[:, :, tc * 512:(tc + 1) * 512], writes=[xk])
                for oc in range(ncols_fm // 128):
                    pt, pk = next_ps(pr)
                    mm_group(P, pt[:], [(w[:, dc, oc * 128:(oc + 1) * 128], xt[:, dc, :]) for dc in range(8)],
                             ["wk", xk], pk)
                    dst, dk = fm_dst(oc, tc)
                    P.op("act", lambda e, d=dst, p=pt: e.activation(out=d, in_=p[:], func=AF.Copy),
                         reads=[pk], writes=[dk])
                for tb in range(4):
                    pt, pk = next_ps(pr)
                    mm_group(P, pt[:], [(xt[:, dc, tb * 128:(tb + 1) * 128], w[:, dc, ncols_fm:ncol]) for dc in range(8)],
                             ["wk", xk], pk)
                    dst, dk = tm_dst(tc * 4 + tb)
                    P.op("dve", lambda e, d=dst, p=pt: e.tensor_copy(out=d, in_=p[:]), reads=[pk], writes=[dk])

        def next_ps(pr):
            t, k = pr.next()
            return t, k

        xTqv = xTq.rearrange("(c p) t -> p c t", p=128)

        with ExitStack() as st:
            kaT = sb(st, "kaT", [128, 4, S], BF16)
            kiT = sb(st, "kiT", [128, S], BF16)
            va = sb(st, "va", [128, 32, 512], BF16)
            with ExitStack() as st0:
                kside(st0, wKA, 640,
                      lambda oc, tc: ((kaT[:, oc, tc * 512:(tc + 1) * 512], "kaT") if oc < 4
                                      else (kiT[:, tc * 512:(tc + 1) * 512], "kiT")),
                      lambda blk: (va[:, blk, :], "va"), "A")
                P.emit()
            wq = sb(st, "wqa", [128, 8, 1024], BF16)
            wwi = sb(st, "wwi", [128, 8, 8], BF16)
            P.dma("pool", wq[:], wQA.rearrange("(c p) n -> p c n", p=128), writes=["wq"])
            P.dma("pool", wwi[:], wWI.rearrange("(c p) n -> p c n", p=128), writes=["wwi"])
            xqb = sb(st, "xqb", [128, 8, TQ], BF16)
            qaT = sb(st, "qaT", [128, 4, TQ], BF16)
            qiT = sb(st, "qiT", [128, 4, TQ], BF16)
            wis = sb(st, "wis", [128, 4, 8], F32)
            diagr = Ring([sb(st, "diag%d" % i, [128, 8, 128], BF16) for i in range(2)], "diag")
            Rr = Ring([sb(st, "R%d" % i, [128, 512], BF16) for i in range(4)], "R")
            Isbr = Ring([sb(st, "Isb%d" % i, [128, S], F32) for i in range(2)], "Isb")
            junk = sb(st, "junk", [128, S], U8)
            mbias = [sb(st, "mbias%d" % i, [128, S], BF16) for i in range(4)]
            cmr = Ring([sb(st, "cm%d" % i, [128, 1024], BF16) for i in range(2)], "cm")
            bis = sb(st, "bis", [128, 8 + NIT + 1], F32)
            bisa = sb(st, "bisa", [128, 8 + NIT + 1], F32)
            t5r = Ring([sb(st, "t5_%d" % i, [128, TQ], BF16) for i in range(4)], "t5")
            pr_ = Ring([sb(st, "p%d" % i, [128, TQ], BF16) for i in range(3)], "p")
            rden = sb(st, "rden", [64, TQ], F32)
            yor = Ring([sb(st, "yo%d" % i, [64, TQ], BF16) for i in range(2)], "yo")
            zar = Ring(psb[0:3], "psb")
            Yr = Ring(psb[4:6], "psY")
            Dr = Ring(psb[6:8], "psD")

            class _IR:
                i = 0

                def next(self):
                    self.i += 1
                    return (psb[3], "psb3") if self.i % 2 else (psb[7], "psD1")
            Ipr = _IR()
            for lg in range(NLG):
                par = lg % 2
                nkb = 8 * (lg + 1)
                Slg = 128 * nkb
                P.dma("pool", xqb[:], xTqv[:, :, lg * TQ:(lg + 1) * TQ], writes=["xqb"])
                for oc in range(8):
                    pt, pk = zar.next()
                    mm_group(P, pt[:], [(wq[:, dc, oc * 128:(oc + 1) * 128], xqb[:, dc, :]) for dc in range(8)],
                             ["wq", "xqb"], pk)
                    if oc < 4:
                        P.op("act", lambda e, d=qaT[:, oc, :], p=pt: e.mul(d, p[:], 0.125),
                             reads=[pk], writes=["qaT"])
                    else:
                        P.op("act", lambda e, d=qiT[:, oc - 4, :], p=pt: e.activation(out=d, in_=p[:], func=AF.Copy),
                             reads=[pk], writes=["qiT"])
                pt, pk = zar.next()
                for tb in range(4):
                    mm_group(P, pt[:, tb * 8:(tb + 1) * 8],
                             [(xqb[:, dc, tb * 128:(tb + 1) * 128], wwi[:, dc, :]) for dc in range(8)],
                             ["wwi", "xqb"], pk)
                P.op("dve", lambda e, p=pt: e.tensor_copy(out=wis[:].rearrange("p a b -> p (a b)"), in_=p[:, 0:32]),
                     reads=[pk], writes=["wis"])
                Istate = {}

                def IDX(tb, lg=lg, par=par, nkb=nkb):
                    dg, dgk = diagr.next()
                    for h in range(8):
                        P.op("pool", lambda e, d=dg[:, h, :], s=wis[:, tb, h:h + 1]: e.tensor_scalar(
                            out=d, in0=ident, scalar1=s, scalar2=None, op0=ALU.mult),
                            reads=["wis", "cb"], writes=[dgk])
                    cm, cmk = cmr.next()
                    P.dma("sp", cm[:], cmask[par, tb], writes=[cmk])
                    Isb, Isbk = Isbr.next()
                    nsc = nkb // 4
                    U = nsc * 8
                    units = [dict(sc=u // 8, h=u % 8) for u in range(U)]
                    Ist = {}

                    def Zf(u):
                        Uu = units[u]
                        h, sc = Uu["h"], Uu["sc"]
                        hp, hc = (h % 2) * 64, h // 2
                        Uu["zt"], Uu["zk"] = zar.next()
                        mm_group(P, Uu["zt"][:], [(qiT[hp:hp + 64, hc, tb * 128:(tb + 1) * 128],
                                                   kiT[hp:hp + 64, sc * 512:(sc + 1) * 512])], ["qiT", "kiT"], Uu["zk"])

                    def Rf(u):
                        Uu = units[u]
                        Uu["rt"], Uu["rk"] = Rr.next()
                        P.op("act", lambda e, d=Uu["rt"], p=Uu["zt"]: e.activation(out=d[:], in_=p[:], func=AF.Relu),
                             reads=[Uu["zk"]], writes=[Uu["rk"]])

                    def Df(u):
                        Uu = units[u]
                        h, sc = Uu["h"], Uu["sc"]
                        if h == 0:
                            Ist[sc] = Ipr.next()
                        Ips, Ik = Ist[sc]
                        mm_group(P, Ips[:], [(dg[:, h, :], Uu["rt"][:])], [dgk, Uu["rk"]], Ik, start=(h == 0), stop=(h == 7))
                        if h == 7:
                            dst = Isb[:, sc * 512:(sc + 1) * 512]
                            if sc >= 2 * lg:
                                c0 = (sc - 2 * lg) * 512
                                P.op("dve", lambda e, d=dst, c=cm[:, c0:c0 + 512], Ips=Ips: e.tensor_tensor(out=d, in0=Ips[:], in1=c, op=ALU.add),
                                     reads=[Ik, cmk], writes=[Isbk])
                            else:
                                P.op("dve", lambda e, d=dst, Ips=Ips: e.tensor_copy(out=d, in_=Ips[:]), reads=[Ik], writes=[Isbk])

                    for u in range(-2, U):
                        if 0 <= u + 2 < U:
                            Zf(u + 2)
                        if 0 <= u + 1 < U:
                            Rf(u + 1)
                        if 0 <= u < U:
                            Df(u)
                    Istate[tb] = (Isb, Isbk)

                def BIS(tb, which, Slg=Slg):
                    Isb, Isbk = Istate[tb]
                    Iv = Isb[:, 0:Slg]
                    mbk = "mbias%d" % tb
                    if which == 0:
                        bs, pfx, jv, jk = bis, "b", junk[:, 0:Slg], "junk"
                    else:
                        bs, pfx, jv, jk = bisa, "a", mbias[tb][:, 0:Slg], mbk
                    K = lambda n: pfx + n
                    L = []
                    A = lambda fn, reads, writes: L.append(lambda: P.op("dve", fn, reads=reads, writes=writes))
                    A(lambda e: e.reduce_max(out=bs[:, 0:1], in_=Iv, axis=mybir.AxisListType.X), [Isbk], [K("B")])
                    A(lambda e: e.tensor_scalar(out=bs[:, 5:6], in0=bs[:, 0:1], scalar1=-1.0, scalar2=None, op0=ALU.mult), [K("B")], [K("N")])
                    A(lambda e: e.tensor_tensor(out=bs[:, 6:7], in0=bs[:, 0:1], in1=bs[:, 5:6], op=ALU.max), [K("B"), K("N")], [K("A")])
                    A(lambda e: e.tensor_scalar(out=bs[:, 7:8], in0=bs[:, 6:7], scalar1=1.0, scalar2=2.0, op0=ALU.max, op1=ALU.mult), [K("A")], [K("R")])
                    A(lambda e: e.tensor_scalar(out=bs[:, 8:8 + NIT + 1], in0=pow2, scalar1=bs[:, 7:8], scalar2=None, op0=ALU.mult), [K("R"), "cf"], [K("steps")])
                    A(lambda e: e.memset(bs[:, 1:2], 0.0), [], [K("cand")])
                    for k in range(NIT):
                        A(lambda e: e.tensor_scalar(out=jv, in0=Iv, scalar1=bs[:, 1:2], scalar2=None, op0=ALU.is_ge, op1=ALU.add, accum_out=bs[:, 2:3]),
                          [Isbk, K("cand")], [K("cnt"), jk])
                        A(lambda e, k=k: e.scalar_tensor_tensor(out=bs[:, 3:4], in0=bs[:, 2:3], scalar=256.0, in1=bs[:, 8 + k:9 + k], op0=ALU.is_ge, op1=ALU.mult),
                          [K("cnt"), K("steps")], [K("inc")])
                        A(lambda e, k=k: e.scalar_tensor_tensor(out=bs[:, 1:2], in0=bs[:, 3:4], scalar=bs[:, 9 + k:10 + k], in1=bs[:, 1:2], op0=ALU.subtract, op1=ALU.add),
                          [K("inc"), K("steps"), K("cand")], [K("cand")])
                    A(lambda e: e.tensor_tensor(out=bs[:, 4:5], in0=bs[:, 1:2], in1=bs[:, 8 + NIT:9 + NIT], op=ALU.subtract), [K("cand"), K("steps")], [K("thr")])
                    A(lambda e: e.tensor_scalar(out=bs[:, 5:6], in0=bs[:, 7:8], scalar1=1.0 - 2.0 ** -(NIT + 1), scalar2=None, op0=ALU.mult), [K("R"), K("A")], [K("M")])
                    A(lambda e: e.tensor_tensor(out=bs[:, 6:7], in0=bs[:, 4:5], in1=bs[:, 5:6], op=ALU.add), [K("thr"), K("M"), K("A")], [K("T1")])
                    A(lambda e: e.tensor_scalar(out=bs[:, 6:7], in0=bs[:, 6:7], scalar1=0.0, scalar2=-1e29, op0=ALU.is_le, op1=ALU.mult), [K("T1")], [K("Pen")])
                    A(lambda e: e.tensor_tensor(out=bs[:, 3:4], in0=bs[:, 4:5], in1=bs[:, 6:7], op=ALU.add), [K("thr"), K("Pen"), K("inc")], [K("thr2")])
                    A(lambda e, d=mbias[tb][:, 0:Slg]: e.tensor_scalar(out=d, in0=Iv, scalar1=bs[:, 3:4], scalar2=NEG, op0=ALU.is_lt, op1=ALU.mult),
                      [Isbk, K("thr2")], [mbk])
                    return L

                for t0 in (0, 2):
                    IDX(t0)
                    IDX(t0 + 1)
                    La, Lb = BIS(t0, 0), BIS(t0 + 1, 1)
                    for fa, fb in zip(La, Lb):
                        fa()
                        fb()
                blocks = [dict(h=h, kb=kb) for h in range(8) for kb in range(nkb)]
                NB = len(blocks)
                YD = {}

                def Af(b):
                    B = blocks[b]
                    h, kb = B["h"], B["kb"]
                    hp, hc = (h % 2) * 64, h // 2
                    near = kb >= 8 * lg - 1
                    B["near"] = near
                    B["at"], B["ak"] = zar.next()
                    at = B["at"]
                    pairs = [(at[:], kaT[hp:hp + 64, hc, kb * 128:(kb + 1) * 128], qaT[hp:hp + 64, hc, :])]
                    rd = ["kaT", "qaT", "cb"] + ["mbias%d" % t for t in range(4)]
                    for tb in range(4):
                        pairs.append((at[:, tb * 128:(tb + 1) * 128], mbias[tb][:, kb * 128:(kb + 1) * 128], ident))
                    if near:
                        t5, t5k = t5r.next()
                        P.dma("sp", t5[:], t5tab[par, h, kb - 8 * lg + 1], writes=[t5k])
                        pairs.append((at[:], ident, t5[:]))
                        rd.append(t5k)
                    mm_group(P, at[:], pairs, rd, B["ak"])

                def Xf(b):
                    B = blocks[b]
                    h = B["h"]
                    B["pt"], B["pk"] = pr_.next()
                    if B["near"]:
                        P.op("act", lambda e, d=B["pt"], a=B["at"]: e.activation(out=d[:], in_=a[:], func=AF.Exp),
                             reads=[B["ak"]], writes=[B["pk"]])
                    else:
                        P.op("act", lambda e, d=B["pt"], a=B["at"], b_=rb31s[:, h:h + 1]: e.activation(out=d[:], in_=a[:], func=AF.Exp, bias=b_),
                             reads=[B["ak"], "rb31s"], writes=[B["pk"]])

                def Vf(b):
                    B = blocks[b]
                    h, kb = B["h"], B["kb"]
                    if kb == 0:
                        YD[h] = (Yr.next(), Dr.next())
                    (Yt, Yk), (Dt, Dk) = YD[h]
                    mm_group(P, Yt[0:64, :], [(va[:, kb, h * 64:(h + 1) * 64], B["pt"][:])], ["va", B["pk"]], Yk,
                             start=(kb == 0), stop=(kb == nkb - 1))
                    mm_group(P, Dt[0:64, :], [(ones[:, 0:64], B["pt"][:])], ["cb", B["pk"]], Dk,
                             start=(kb == 0), stop=(kb == nkb - 1))
                    if kb == nkb - 1:
                        P.op("dve", lambda e, d=Dt: e.reciprocal(out=rden[:], in_=d[0:64, :]), reads=[Dk], writes=["rden"])
                        yo, yok = yor.next()
                        P.op("dve", lambda e, y=Yt, o=yo: e.tensor_tensor(out=o[:], in0=y[0:64, :], in1=rden[:], op=ALU.mult),
                             reads=[Yk, "rden"], writes=[yok])
                        P.dma("sp", ya_d[h * 64:(h + 1) * 64, lg * TQ:(lg + 1) * TQ], yo[:], reads=[yok], writes=["ya_d%d_%d" % (lg, h)])

                for i in range(-2, NB):
                    if 0 <= i + 2 < NB:
                        Af(i + 2)
                    if 0 <= i + 1 < NB:
                        Xf(i + 1)
                    if 0 <= i < NB:
                        Vf(i)
            P.emit()

        with ExitStack() as st:
            kbT = sb(st, "kbT", [128, 4, S], BF16)
            vb = sb(st, "vb", [128, 32, 512], BF16)
            with ExitStack() as st0:
                kside(st0, wKB, 512, lambda oc, tc: (kbT[:, oc, tc * 512:(tc + 1) * 512], "kbT"),
                      lambda blk: (vb[:, blk, :], "vb"), "B")
                P.emit()
            wq = sb(st, "wqb", [128, 8, 512], BF16)
            P.dma("pool", wq[:], wQB.rearrange("(c p) n -> p c n", p=128), writes=["wq"])
            xqb = sb(st, "xqb2", [128, 8, TQ], BF16)
            qbT = sb(st, "qbT", [128, 4, TQ], BF16)
            sbm = sb(st, "sbm", [128, 8, TQ], BF16)
            er = Ring([sb(st, "e%d" % i, [128, TQ], F32) for i in range(2)], "e")
            spr = Ring([sb(st, "sp%d" % i, [128, TQ], BF16) for i in range(3)], "spl")
            wr_ = Ring([sb(st, "w%d" % i, [128, TQ], BF16) for i in range(3)], "w")
            accr = Ring([sb(st, "acc%d" % i, [128, TQ], BF16) for i in range(2)], "acc")
            yor = Ring([sb(st, "yob%d" % i, [64, TQ], BF16) for i in range(2)], "yob")
            ar = Ring(psb[0:4], "psb")
            Yr = Ring(psb[4:6], "psY")
            for lg in range(NLG):
                par = lg % 2
                nkb = 8 * (lg + 1)
                P.dma("pool", xqb[:], xTqv[:, :, lg * TQ:(lg + 1) * TQ], writes=["xqb"])
                P.dma("sp", sbm[:], sbmask[par].rearrange("j p t -> p j t"), writes=["sbm"])
                for oc in range(4):
                    pt, pk = ar.next()
                    mm_group(P, pt[:], [(wq[:, dc, oc * 128:(oc + 1) * 128], xqb[:, dc, :]) for dc in range(8)],
                             ["wq", "xqb"], pk)
                    P.op("act", lambda e, d=qbT[:, oc, :], p=pt: e.mul(d, p[:], 0.125),
                         reads=[pk], writes=["qbT"])
                blocks = []
                for h in range(8):
                    for idx, kb in enumerate(range(nkb - 1, -1, -1)):
                        blocks.append(dict(h=h, idx=idx, kb=kb, first=(idx == 0), last=(idx == nkb - 1)))
                NB = len(blocks)
                Ystate = {}

                def S1(b):
                    B = blocks[b]
                    h, kb = B["h"], B["kb"]
                    hp, hc = (h % 2) * 64, h // 2
                    B["at"], B["ak"] = ar.next()
                    pairs = [(kbT[hp:hp + 64, hc, kb * 128:(kb + 1) * 128], qbT[hp:hp + 64, hc, :])]
                    rd = ["kbT", "qbT"]
                    if kb >= 8 * lg:
                        pairs.append((ident, sbm[:, kb - 8 * lg, :]))
                        rd += ["cb", "sbm"]
                    mm_group(P, B["at"][:], pairs, rd, B["ak"], stop=False)

                def E1(b):
                    B = blocks[b]
                    B["et"], B["ek"] = er.next()
                    P.op("act", lambda e, d=B["et"], a=B["at"]: e.activation(out=d[:], in_=a[:], func=AF.Exp), reads=[B["ak"]], writes=[B["ek"]], big=True)

                def Lacc(b):
                    B = blocks[b]
                    et, ek = B["et"], B["ek"]
                    B["sp"], B["spk"] = spr.next()
                    P.op("act", lambda e, d=B["sp"], a=et: e.activation(out=d[:], in_=a[:], func=AF.Ln, bias=1.0, scale=1.0),
                         reads=[ek], writes=[B["spk"]], big=True)
                    if not B["last"]:
                        acn, acnk = accr.next()
                        if B["first"]:
                            P.op("pool", lambda e, d=acn, s_=B["sp"]: e.tensor_copy(out=d[:], in_=s_[:]), reads=[B["spk"]], writes=[acnk])
                        else:
                            pa_, pak_ = B["acc"]
                            P.op("pool", lambda e, d=acn, s_=B["sp"], a=pa_: e.tensor_tensor(out=d[:], in0=a[:], in1=s_[:], op=ALU.add),
                                 reads=[B["spk"], pak_], writes=[acnk])
                        blocks[b + 1]["acc"] = (acn, acnk)

                def S2(b):
                    B = blocks[b]
                    pairs = [(negU, B["sp"][:])]
                    rd = ["cb", B["spk"]]
                    if not B["first"]:
                        pairs.append((negones, B["acc"][0][:]))
                        rd.append(B["acc"][1])
                    mm_group(P, B["at"][:], pairs, rd, B["ak"], start=False)

                def E2(b):
                    B = blocks[b]
                    B["w"], B["wk"] = wr_.next()
                    P.op("act", lambda e, d=B["w"], a=B["at"]: e.activation(out=d[:], in_=a[:], func=AF.Exp), reads=[B["ak"]], writes=[B["wk"]], big=True)

                def S3(b):
                    B = blocks[b]
                    h, kb = B["h"], B["kb"]
                    if B["first"]:
                        Ystate[h] = Yr.next()
                    Yt, Yk = Ystate[h]
                    mm_group(P, Yt[0:64, :], [(vb[:, kb, h * 64:(h + 1) * 64], B["w"][:])], ["vb", B["wk"]], Yk,
                             start=B["first"], stop=B["last"])
                    if B["last"]:
                        yo, yok = yor.next()
                        P.op("dve", lambda e, y=Yt, o=yo: e.tensor_copy(out=o[:], in_=y[0:64, :]), reads=[Yk], writes=[yok])
                        P.dma("sp", yb_d[h * 64:(h + 1) * 64, lg * TQ:(lg + 1) * TQ], yo[:], reads=[yok], writes=["yb_d%d_%d" % (lg, h)])

                for i in range(-2, NB + 1):
                    if 0 <= i < NB:
                        S2(i)
                    if 0 <= i + 2 < NB:
                        S1(i + 2)
                    if 0 <= i - 1 < NB:
                        S3(i - 1)
                    if 0 <= i + 1 < NB:
                        E1(i + 1)
                    if 0 <= i < NB:
                        E2(i)
                    if 0 <= i + 1 < NB:
                        Lacc(i + 1)
            P.emit()

        with ExitStack() as st:
            wg = sb(st, "wg", [128, 8, 2048], BF16)
            wa = sb(st, "wa", [128, 4, D], BF16)
            wb = sb(st, "wb", [128, 4, D], BF16)
            wo = sb(st, "wo", [128, 8, D], BF16)
            wr = sb(st, "wr", [128, 8, NE], F32)
            brt = sb(st, "brt", [128, NE], F32)
            lnps = sb(st, "lnps", [128, 2, D], F32)
            P.dma("pool", wg[:], wG.rearrange("(c p) n -> p c n", p=128), writes=["wg"])
            P.dma("pool", wa[:], wA.rearrange("(c p) n -> p c n", p=128), writes=["wa"])
            P.dma("pool", wb[:], wB.rearrange("(c p) n -> p c n", p=128), writes=["wb"])
            P.dma("pool", wo[:], wO.rearrange("(c p) n -> p c n", p=128), writes=["wo"])
            P.dma("sp", wr[:], wR.rearrange("(c p) n -> p c n", p=128), writes=["wr"])
            P.dma("sp", brt[:], bRT[:, :], writes=["brt"])
            P.dma("sp", lnps[:], lnp[0:2].rearrange("a p d -> p a d"), writes=["lnps"])
            xqb = sb(st, "xqb3", [128, 8, TQ], BF16)
            yas = sb(st, "yas", [128, 4, TQ], BF16)
            ybs = sb(st, "ybs", [128, 4, TQ], BF16)
            sg = sb(st, "sg", [128, 16, TQ], BF16)
            mT = sb(st, "mT", [128, 8, TQ], BF16)
            t1 = sb(st, "t1", [128, TQ], F32)
            t2 = sb(st, "t2", [128, TQ], F32)
            xres = sb(st, "xres", [128, D], F32)
            r1 = sb(st, "r1", [128, D], F32)
            sq = sb(st, "sq", [128, D], F32)
            h1t = sb(st, "h1t", [128, D], F32)
            h1T = sb(st, "h1T", [128, 8, 128], F32)
            sm = sb(st, "sm", [128, 64], F32)
            pr = Ring(psb, "psb")
            for lg in range(NLG):
                P.dma("pool", xqb[:], xTqv[:, :, lg * TQ:(lg + 1) * TQ], writes=["xqb"])
                P.dma("sp", yas[:], ya_d[:, lg * TQ:(lg + 1) * TQ].rearrange("(c p) t -> p c t", p=128), writes=["yas"])
                P.dma("sp", ybs[:], yb_d[:, lg * TQ:(lg + 1) * TQ].rearrange("(c p) t -> p c t", p=128), writes=["ybs"])
                for oc in range(16):
                    pt, pk = pr.next()
                    mm_group(P, pt[:], [(wg[:, dc, oc * 128:(oc + 1) * 128], xqb[:, dc, :]) for dc in range(8)], ["wg", "xqb"], pk)
                    P.op("act", lambda e, d=sg[:, oc, :], p=pt: e.activation(out=d, in_=p[:], func=AF.Sigmoid), reads=[pk], writes=["sg"])
                for oc in range(8):
                    pa, pak = pr.next()
                    mm_group(P, pa[:], [(wa[:, fc, oc * 128:(oc + 1) * 128], yas[:, fc, :]) for fc in range(4)], ["wa", "yas"], pak)
                    pb, pbk = pr.next()
                    mm_group(P, pb[:], [(wb[:, fc, oc * 128:(oc + 1) * 128], ybs[:, fc, :]) for fc in range(4)], ["wb", "ybs"], pbk)
                    P.op("dve", lambda e, p=pa, g=sg[:, oc, :]: e.tensor_tensor(out=t1[:], in0=p[:], in1=g, op=ALU.mult), reads=[pak, "sg"], writes=["t1"])
                    P.op("dve", lambda e, p=pb, g=sg[:, 8 + oc, :]: e.tensor_tensor(out=t2[:], in0=p[:], in1=g, op=ALU.mult), reads=[pbk, "sg"], writes=["t2"])
                    P.op("pool", lambda e, d=mT[:, oc, :]: e.tensor_tensor(out=d, in0=t1[:], in1=t2[:], op=ALU.add), reads=["t1", "t2"], writes=["mT"])
                for tb in range(4):
                    blk = lg * 4 + tb
                    P.dma("sp", xres[:], xq[blk * 128:(blk + 1) * 128, :], writes=["xres"])
                    for half in range(2):
                        pm, pmk = pr.next()
                        mm_group(P, pm[:], [(mT[:, dc, tb * 128:(tb + 1) * 128], wo[:, dc, half * 512:(half + 1) * 512]) for dc in range(8)],
                                 ["mT", "wo"], pmk)
                        P.op("dve", lambda e, p=pm, hs=slice(half * 512, (half + 1) * 512): e.scalar_tensor_tensor(
                            out=r1[:, hs], in0=xres[:, hs], scalar=ALPHA, in1=p[:], op0=ALU.mult, op1=ALU.add),
                            reads=[pmk, "xres"], writes=["r1"])
                    layer_norm(P, r1, sq, sm, lnps, h1t, "r1", "h1t")
                    P.dma("sp", h1_d[blk * 128:(blk + 1) * 128, :], h1t[:], reads=["h1t"], writes=["h1_d%d" % blk])
                    for half in range(2):
                        ptt, ptk = pr.next()
                        mm_group(P, ptt[:], [(ptt[:, j * 128:(j + 1) * 128], h1t[:, (half * 4 + j) * 128:(half * 4 + j + 1) * 128], identf)
                                             for j in range(4)], ["h1t", "cf"], ptk)
                        P.op("act", lambda e, p=ptt, d=h1T[:, half * 4:(half + 1) * 4, :]: e.activation(
                            out=d.rearrange("p a b -> p (a b)"), in_=p[:], func=AF.Copy), reads=[ptk], writes=["h1T"])
                    prr, prk = pr.next()
                    mm_group(P, prr[:, 0:NE], [(h1T[:, dc, :], wr[:, dc, :]) for dc in range(8)], ["h1T", "wr"], prk)

                    lgt = sm[:, 16:48]
                    P.op("dve", lambda e, p=prr: e.tensor_tensor(out=sm[:, 16:48], in0=p[:, 0:NE], in1=brt[:], op=ALU.add),
                         reads=[prk, "brt"], writes=["rlg"])
                    P.op("dve", lambda e: e.max(out=sm[:, 0:8], in_=sm[:, 16:48]), reads=["rlg"], writes=["rtop"])
                    P.op("dve", lambda e, blk=blk: e.tensor_scalar(out=mask_f[:, blk, :], in0=sm[:, 16:48], scalar1=sm[:, 3:4], scalar2=None, op0=ALU.is_ge),
                         reads=["rlg", "rtop"], writes=["maskf"])
                    P.op("dve", lambda e, blk=blk: e.tensor_copy(out=mask_b[:, blk, :], in_=mask_f[:, blk, :]), reads=["maskf"], writes=["maskb"])
                    P.op("dve", lambda e: e.tensor_scalar(out=sm[:, 48:49], in0=sm[:, 0:1], scalar1=-1.0, scalar2=None, op0=ALU.mult),
                         reads=["rtop"], writes=["rneg"])
                    P.op("act", lambda e: e.activation(out=sm[:, 16:48], in_=sm[:, 16:48], func=AF.Exp, bias=sm[:, 48:49]),
                         reads=["rlg", "rneg", "maskf"], writes=["rex"])
                    P.op("dve", lambda e, blk=blk: e.tensor_tensor(out=sm[:, 16:48], in0=sm[:, 16:48], in1=mask_f[:, blk, :], op=ALU.mult),
                         reads=["rex", "maskf"], writes=["rexm"])
                    P.op("dve", lambda e: e.reduce_sum(out=sm[:, 8:9], in_=sm[:, 16:48], axis=mybir.AxisListType.X), reads=["rexm"], writes=["rden"])
                    P.op("dve", lambda e: e.reciprocal(out=sm[:, 9:10], in_=sm[:, 8:9]), reads=["rden"], writes=["rrd"])
                    P.op("dve", lambda e, blk=blk: e.tensor_scalar(out=gates_all[:, blk, :], in0=sm[:, 16:48], scalar1=sm[:, 9:10], scalar2=None, op0=ALU.mult),
                         reads=["rexm", "rrd"], writes=["gates", "rlg"])
            P.emit()

        with ExitStack() as stf:
          f = sb(stf, "f", [128, NBLK, D], F32)
          with ExitStack() as st:
            h1b = sb(st, "h1b", [128, NBLK, D], BF16)
            P.dma("pool", h1b[:], h1_d.rearrange("(b p) d -> p b d", p=128), reads=["h1_d"], writes=["h1b"])
            P.op("pool", lambda e: e.memset(f[:], 0.0), writes=["f%d" % b for b in range(NBLK)])
            wgur = Ring([sb(st, "wgu%d" % i, [128, 8, D], BF16) for i in range(2)], "wgu")
            wdnr = Ring([sb(st, "wdn%d" % i, [128, 4, D], BF16) for i in range(2)], "wdn")
            bgu = sb(st, "bgu", [128, NE * 16], F32)
            P.dma("sp", bgu[:], bGU[:, :], writes=["bgu"])
            posm = sb(st, "posm", [128, NBLK, NE], F32)
            Se = sb(st, "Se", [128, NBLK, CAP], BF16)
            STe = sb(st, "STe", [128, 3, NOWN], BF16)
            xg = sb(st, "xg", [128, 8, CAP], BF16)
            actT = sb(st, "actT", [128, 8, CAP], BF16)
            Ysb = sb(st, "Ysb", [128, 3, D], BF16)
            tar = Ring([sb(st, "ta%d" % i, [128, CAP], F32) for i in range(2)], "ta")
            tsr = Ring([sb(st, "tsg%d" % i, [128, CAP], F32) for i in range(2)], "tsg")
            tur = Ring([sb(st, "tu%d" % i, [128, CAP], F32) for i in range(2)], "tu")
            tgr = Ring([sb(st, "tg%d" % i, [128, CAP], F32) for i in range(2)], "tg")
            bgs = sb(st, "bgs", [128, NE * 16], F32)
            P.op("dve", lambda e: e.tensor_scalar(out=bgs[:], in0=bgu[:], scalar1=1.702, scalar2=None, op0=ALU.mult), reads=["bgu"], writes=["bgs"])
            pr = Ring(psb, "psb")
            for blk in range(NBLK):
                pt, pk = pr.next()
                pairs = [(ustrict, mask_b[:, blk, :])] + [(ones, mask_b[:, b2, :]) for b2 in range(blk)]
                mm_group(P, pt[:, 0:NE], pairs, ["cb", "maskb"], pk)

                P.op("dve", lambda e, p=pt, blk=blk: e.scalar_tensor_tensor(out=posm[:, blk, :], in0=p[:, 0:NE], scalar=1.0, in1=mask_f[:, blk, :],
                                                                           op0=ALU.add, op1=ALU.mult), reads=[pk, "maskf"], writes=["posm1"])
                P.op("dve", lambda e, blk=blk: e.tensor_scalar(out=posm[:, blk, :], in0=posm[:, blk, :], scalar1=-1.0, scalar2=None, op0=ALU.add),
                     reads=["posm1"], writes=["posm"])
            def build_Se(ex):
                for blk in range(NBLK):
                    P.op("dve", lambda e, d=Se[:, blk, :], s=posm[:, blk, ex:ex + 1]: e.tensor_scalar(
                        out=d, in0=iota, scalar1=s, scalar2=None, op0=ALU.is_equal), reads=["posm", "cf"], writes=["Se"])

            WG, WD = {}, {}

            def LOAD_GU(ex):
                WG[ex] = []
                for half in range(2):
                    t, k = wgur.next()
                    P.dma("pool", t[:], wGU[ex, :, half * D:(half + 1) * D].rearrange("(c p) n -> p c n", p=128), writes=[k])
                    WG[ex].append((t, k))

            def LOAD_DN(ex):
                WD[ex] = []
                for half in range(2):
                    t, k = wdnr.next()
                    P.dma("pool", t[:], wDN[ex, half * 512:(half + 1) * 512, :].rearrange("(c p) n -> p c n", p=128), writes=[k])
                    WD[ex].append((t, k))

            def ST(ex):
                for ci, (c0, cs) in enumerate(CBS):
                    for tq in range(4):
                        pt, pk = pr.next()
                        mm_group(P, pt[:], [(pt[0:cs, j * 128:(j + 1) * 128], Se[:, tq * 4 + j, c0:c0 + cs], ident) for j in range(4)],
                                 ["Se", "cb"], pk)
                        P.op("act", lambda e, p=pt, d=STe[0:cs, ci, tq * 512:(tq + 1) * 512], cs=cs: e.activation(out=d, in_=p[0:cs, :], func=AF.Copy),
                             reads=[pk], writes=["STe"])

            def GATHER(ex):
                for dc in range(8):
                    pt, pk = pr.next()
                    mm_group(P, pt[:, 0:CAP], [(h1b[:, blk, dc * 128:(dc + 1) * 128], Se[:, blk, :]) for blk in range(NBLK)],
                             ["h1b", "Se"], pk)
                    P.op("act", lambda e, p=pt, d=xg[:, dc, :]: e.activation(out=d, in_=p[:, 0:CAP], func=AF.Copy), reads=[pk], writes=["xg"])

            def GATEUP(ex):
                wa_, wak = WG[ex][0]
                wu_, wuk = WG[ex][1]
                for fc in range(8):
                    pa_, pak = pr.next()
                    mm_group(P, pa_[:, 0:CAP], [(wa_[:, dc, fc * 128:(fc + 1) * 128], xg[:, dc, :]) for dc in range(8)], [wak, "xg"], pak)
                    pu_, puk = pr.next()
                    mm_group(P, pu_[:, 0:CAP], [(wu_[:, dc, fc * 128:(fc + 1) * 128], xg[:, dc, :]) for dc in range(8)], [wuk, "xg"], puk)
                    ca = ex * 16 + fc
                    cu = ex * 16 + 8 + fc
                    ta, tak = tar.next()
                    tsg, tsk = tsr.next()
                    tu, tuk = tur.next()
                    tg, tgk = tgr.next()
                    P.op("dve", lambda e, p=pa_, b=bgu[:, ca:ca + 1], d=ta: e.tensor_scalar(out=d[:], in0=p[:, 0:CAP], scalar1=b, scalar2=7.0, op0=ALU.add, op1=ALU.min),
                         reads=[pak, "bgu"], writes=[tak])
                    P.op("act", lambda e, a=ta, d=tsg: e.activation(out=d[:], in_=a[:], func=AF.Sigmoid, scale=1.702),
                         reads=[tak], writes=[tsk])
                    P.op("dve", lambda e, p=pu_, b=bgu[:, cu:cu + 1], d=tu: e.tensor_scalar(out=d[:], in0=p[:, 0:CAP], scalar1=b, scalar2=7.0, op0=ALU.add, op1=ALU.min),
                         reads=[puk, "bgu"], writes=[tuk])
                    P.op("pool", lambda e, d=tg, a=ta, g=tsg: e.tensor_tensor(out=d[:], in0=a[:], in1=g[:], op=ALU.mult), reads=[tak, tsk], writes=[tgk])
                    P.op("dve", lambda e, d=tu: e.tensor_scalar(out=d[:], in0=d[:], scalar1=-7.0, scalar2=1.0, op0=ALU.max, op1=ALU.add),
                         reads=[tuk], writes=[tuk])
                    P.op("pool", lambda e, d=actT[:, fc, :], u=tu, g=tg: e.tensor_tensor(out=d, in0=u[:], in1=g[:], op=ALU.mult),
                         reads=[tuk, tgk], writes=["actT"])

            def DOWN(ex):
                wd_t = WD[ex]
                for ci, (c0, cs) in enumerate(CBS):
                    for half in range(2):
                        pt, pk = pr.next()
                        mm_group(P, pt[0:cs, :], [(actT[:, fc, c0:c0 + cs], wd_t[fc // 4][0][:, fc % 4, half * 512:(half + 1) * 512]) for fc in range(8)],
                                 ["actT", wd_t[0][1], wd_t[1][1]], pk)
                        P.op("act", lambda e, p=pt, d=Ysb[0:cs, ci, half * 512:(half + 1) * 512], cs=cs: e.activation(out=d, in_=p[0:cs, :], func=AF.Copy),
                             reads=[pk], writes=["Ysb"])

            def SCATTER(ex):
                for blk in range(NBLK):
                    for half in range(2):
                        pt, pk = pr.next()
                        mm_group(P, pt[:], [(STe[0:cs, ci, blk * 128:(blk + 1) * 128], Ysb[0:cs, ci, half * 512:(half + 1) * 512])
                                            for ci, (c0, cs) in enumerate(CBS)], ["STe", "Ysb"], pk)
                        fs = f[:, blk, half * 512:(half + 1) * 512]
                        P.op("dve", lambda e, p=pt, fs=fs, g=gates_all[:, blk, ex:ex + 1]: e.scalar_tensor_tensor(
                            out=fs, in0=p[:], scalar=g, in1=fs, op0=ALU.mult, op1=ALU.add),
                            reads=[pk, "gates", "f%d" % blk], writes=["f%d" % blk])

            LOAD_GU(0)
            LOAD_DN(0)
            build_Se(0)
            ST(0)
            GATHER(0)
            for ex in range(NE):
                if ex + 1 < NE:
                    build_Se(ex + 1)
                GATEUP(ex)
                if ex + 1 < NE:
                    LOAD_GU(ex + 1)
                    GATHER(ex + 1)
                DOWN(ex)
                if ex + 1 < NE:
                    LOAD_DN(ex + 1)
                SCATTER(ex)
                if ex + 1 < NE:
                    ST(ex + 1)
            if debug:
                P.dma("sp", dbg_f, f[:], reads=["f%d" % b for b in range(NBLK)], writes=["dbg_f"])
                P.dma("sp", dbg_g, gates_all[:], reads=["gates"], writes=["dbg_g"])
                P.dma("sp", dbg_p, posm[:], reads=["posm"], writes=["dbg_p"])
                P.dma("sp", dbg_x, xg[:], reads=["xg"], writes=["dbg_x"])
                P.dma("sp", dbg_a, actT[:], reads=["actT"], writes=["dbg_a"])
                P.dma("sp", dbg_y, Ysb[:], reads=["Ysb"], writes=["dbg_y"])
            P.emit()
          with ExitStack() as st:
            pr = Ring(psb, "psb")
            bdn = sb(st, "bdn", [NE, D], BF16)
            lnps = sb(st, "lnps2", [128, 2, D], F32)
            P.dma("pool", bdn[:], bDN[:, :], writes=["bdn"])
            P.dma("sp", lnps[:], lnp[2:4].rearrange("a p d -> p a d"), writes=["lnps"])
            h1r = Ring([sb(st, "h1r%d" % i, [128, D], F32) for i in range(2)], "h1r")
            r2 = sb(st, "r2", [128, D], F32)
            sq = sb(st, "sq2", [128, D], F32)
            sm = sb(st, "sm2", [128, 64], F32)
            gT = sb(st, "gT", [NE, 128], BF16)
            outr = Ring([sb(st, "ot%d" % i, [128, D], F32) for i in range(2)], "ot")
            for blk in range(NBLK):
                ht, hk = h1r.next()
                P.dma("sp", ht[:], h1_d[blk * 128:(blk + 1) * 128, :], reads=["h1_d"], writes=[hk])
                pt, pk = pr.next()
                mm_group(P, pt[0:NE, 0:128], [(gates_all[:, blk, :], identf)], ["gates", "cf"], pk)
                P.op("act", lambda e, p=pt: e.activation(out=gT[:], in_=p[0:NE, 0:128], func=AF.Copy), reads=[pk], writes=["gT"])
                for half in range(2):
                    hs = slice(half * 512, (half + 1) * 512)
                    pb_, pbk = pr.next()
                    mm_group(P, pb_[:], [(gT[:], bdn[:, hs])], ["gT", "bdn"], pbk)
                    P.op("dve", lambda e, hs=hs, ht=ht, blk=blk: e.scalar_tensor_tensor(
                        out=r2[:, hs], in0=ht[:, hs], scalar=ALPHA, in1=f[:, blk, hs], op0=ALU.mult, op1=ALU.add),
                        reads=[hk, "f%d" % blk], writes=["r2"])
                    P.op("dve", lambda e, hs=hs, p=pb_: e.tensor_tensor(out=r2[:, hs], in0=r2[:, hs], in1=p[:], op=ALU.add),
                         reads=[pbk, "r2"], writes=["r2"])
                ot, otk = outr.next()
                layer_norm(P, r2, sq, sm, lnps, ot, "r2", otk)
                P.dma("sp", out[blk * 128:(blk + 1) * 128, :], ot[:], reads=[otk], writes=["out%d" % blk])
            P.emit()
    return nc


def layer_norm(P, src, sq, sm, lnps, dst, skey, dkey):
    invn = 1.0 / D
    P.op("act", lambda e: e.activation(out=sq[:], in_=src[:], func=AF.Square), reads=[skey], writes=["sq"])
    P.op("dve", lambda e: e.tensor_scalar(out=dst[:], in0=src[:], scalar1=invn, scalar2=None, op0=ALU.mult, op1=ALU.add, accum_out=sm[:, 56:57]),
         reads=[skey], writes=["lmean", dkey])
    P.op("dve", lambda e: e.tensor_scalar(out=dst[:], in0=sq[:], scalar1=invn, scalar2=None, op0=ALU.mult, op1=ALU.add, accum_out=sm[:, 57:58]),
         reads=["sq"], writes=["lex2", dkey])
    P.op("dve", lambda e: e.tensor_tensor(out=sm[:, 58:59], in0=sm[:, 56:57], in1=sm[:, 56:57], op=ALU.mult), reads=["lmean"], writes=["lm2"])
    P.op("dve", lambda e: e.tensor_tensor(out=sm[:, 59:60], in0=sm[:, 57:58], in1=sm[:, 58:59], op=ALU.subtract), reads=["lex2", "lm2"], writes=["lvar0"])
    P.op("dve", lambda e: e.tensor_scalar(out=sm[:, 62:63], in0=sm[:, 59:60], scalar1=0.0, scalar2=LN_EPS, op0=ALU.max, op1=ALU.add),
         reads=["lvar0"], writes=["lvar"])
    P.op("act", lambda e: e.activation(out=sm[:, 60:61], in_=sm[:, 62:63], func=AF.Sqrt), reads=["lvar"], writes=["lstd"])
    P.op("dve", lambda e: e.reciprocal(out=sm[:, 61:62], in_=sm[:, 60:61]), reads=["lstd"], writes=["lrstd"])
    P.op("dve", lambda e: e.tensor_scalar(out=dst[:], in0=src[:], scalar1=sm[:, 56:57], scalar2=sm[:, 61:62], op0=ALU.subtract, op1=ALU.mult),
         reads=["lmean", "lrstd", skey], writes=[dkey])
    P.op("dve", lambda e: e.tensor_tensor(out=dst[:], in0=dst[:], in1=lnps[:, 0, :], op=ALU.mult), reads=[dkey, "lnps"], writes=[dkey])
    P.op("dve", lambda e: e.tensor_tensor(out=dst[:], in0=dst[:], in1=lnps[:, 1, :], op=ALU.add), reads=[dkey, "lnps"], writes=[dkey])


def _t5_bucket(rel):
    n = np.maximum(rel, 0)
    nf = np.maximum(n, 1).astype(np.float32)
    large = 16 + (np.log(nf / np.float32(16)) / np.float32(math.log(128 / 16)) * np.float32(16)).astype(np.int32)
    large = np.minimum(large, 31)
    return np.where(n < 16, n, large)


def _host_tables(rel_bias, hf):
    ss = np.arange(128)[:, None]
    tt = np.arange(TQ)[None, :]
    offs = [0, 4] if hf == 0 else [4, 0]
    t5 = np.empty((2, 8, 9, 128, TQ), np.float32)
    sbm = np.empty((2, 8, 128, TQ), np.float32)
    cm = np.empty((2, 4, 128, 1024), np.float32)
    for par, off in enumerate(offs):
        for j in range(8):
            rel = 128 * (off - j) + tt - ss
            sbm[par, j] = np.where(rel > 0, 0.0, NEG)
        for jj in range(9):
            rel = 128 * (off - (jj - 1)) + tt - ss
            bk = _t5_bucket(rel)
            for h in range(8):
                t5[par, h, jj] = np.where(rel >= 0, rel_bias[bk, h], NEG)
        for tb in range(4):
            tl = np.arange(128)[:, None]
            sg = np.arange(1024)[None, :]
            rel = 128 * off + 128 * tb + tl - sg
            cm[par, tb] = np.where(rel >= 0, 0.0, -1e30)
    return t5.astype(bf16), sbm.astype(bf16), cm.astype(bf16)


_NC_CACHE = {}
_DEBUG = False


def kernel(x, w_in, w_branch_a, w_branch_b, w_out, rel_bias, ln1_g, ln1_b, w_router, b_router,
           w_gate_up, b_gate_up, w_down, b_down, ln2_g, ln2_b):
    x = np.asarray(x, np.float32)
    w = np.asarray(w_in, np.float32)[0]
    cs = np.cumsum([0, 512, 512, 512, 512, 64, 8, 512, 512, 512, 1024, 1024])
    qa, ka, va, qi, ki, wi, qb, kb, vb, ga, gb = [w[:, cs[i]:cs[i + 1]] for i in range(11)]
    shared = dict(
        wKA=np.ascontiguousarray(np.concatenate([ka, ki, ki, va], 1)),
        wKB=np.ascontiguousarray(np.concatenate([kb, vb], 1)),
        wQA=np.ascontiguousarray(np.concatenate([qa, qi], 1)),
        wWI=np.ascontiguousarray(wi),
        wQB=np.ascontiguousarray(qb),
        wG=np.ascontiguousarray(np.concatenate([ga, gb], 1)),
        wA=np.asarray(w_branch_a, np.float32)[0], wB=np.asarray(w_branch_b, np.float32)[0],
        wO=np.asarray(w_out, np.float32)[0], wR=np.asarray(w_router, np.float32)[0],
        wGU=np.asarray(w_gate_up, np.float32)[0], wDN=np.asarray(w_down, np.float32)[0],
        bGU=np.ascontiguousarray(np.asarray(b_gate_up, np.float32)[0].reshape(NE, 16, 128).transpose(2, 0, 1).reshape(128, NE * 16)),
        bDN=np.asarray(b_down, np.float32)[0],
        bRT=np.ascontiguousarray(np.broadcast_to(np.asarray(b_router, np.float32)[0][None, :], (128, NE))),
        lnp=np.ascontiguousarray(np.stack([np.broadcast_to(np.asarray(a, np.float32)[0][None, :], (128, D))
                                           for a in (ln1_g, ln1_b, ln2_g, ln2_b)])),
        rb31=np.ascontiguousarray(np.broadcast_to(np.asarray(rel_bias, np.float32)[31][None, :], (128, 8))),
    )
    eye = np.eye(128, dtype=np.float32)
    jj = np.arange(128)[:, None]
    ss = np.arange(128)[None, :]
    negU = np.where(jj >= ss, -1.0, 0.0)
    ustrict = np.where(jj < ss, 1.0, 0.0)
    shared["cst_b"] = np.concatenate([eye, negU, -np.ones((128, 128)), ustrict, np.ones((128, 128))], 1).astype(bf16)
    pow2 = np.broadcast_to((2.0 ** -np.arange(NIT + 1))[None, :], (128, NIT + 1))
    iota = np.broadcast_to(np.arange(CAP)[None, :], (128, CAP))
    shared["cst_f"] = np.ascontiguousarray(np.concatenate([eye, pow2, iota], 1).astype(np.float32))
    rb = np.asarray(rel_bias, np.float32)
    tabs = {hf: _host_tables(rb, hf) for hf in (0, 1)}
    in_maps = []
    for c in range(NCORES):
        b, hf = c // 2, c % 2
        cols = np.concatenate([np.arange(g * TQ, (g + 1) * TQ) for g in GROUPS[hf]])
        xb = x[b]
        m = dict(shared)
        m["xT"] = np.ascontiguousarray(xb.T)
        m["xTq"] = np.ascontiguousarray(xb[cols].T)
        m["xq"] = np.ascontiguousarray(xb[cols])
        m["t5tab"], m["sbmask"], m["cmask"] = tabs[hf]
        in_maps.append(m)
    if "nc" not in _NC_CACHE:
        _NC_CACHE["nc"] = build_program(debug=_DEBUG)
    res = run_bass_kernel_spmd(_NC_CACHE["nc"], in_maps, core_ids=list(range(NCORES)))
    if _DEBUG:
        _NC_CACHE["res"] = res
    outp = np.empty((4, S, D), np.float32)
    for c in range(NCORES):
        b, hf = c // 2, c % 2
        cols = np.concatenate([np.arange(g * TQ, (g + 1) * TQ) for g in GROUPS[hf]])
        outp[b, cols] = res.results[c]["out"]
    return outp
```

```python
import math
from contextlib import ExitStack

import numpy as np
import ml_dtypes

import concourse.bass as bass
import concourse.mybir as mybir
from concourse.bass_utils import run_bass_kernel_spmd

F32 = mybir.dt.float32
BF16 = mybir.dt.bfloat16
U8 = mybir.dt.uint8
ALU = mybir.AluOpType
AF = mybir.ActivationFunctionType
bf16 = ml_dtypes.bfloat16

NCORES = 8
S = 4096
D = 1024
NLG = 4
TQ = 512
NOWN = NLG * TQ
NBLK = NOWN // 128
NE = 32
CAP = 320
CBS = [(0, 128), (128, 128), (256, 64)]
NIT = 14
NEG = -30000.0
ALPHA = 2.0 ** 0.25
LN_EPS = 1e-5
GROUPS = {0: [0, 3, 4, 7], 1: [1, 2, 5, 6]}

ENGS = ("pe", "act", "dve", "pool", "sp")
NDMASEM = 12


class Prog:
    def __init__(self, nc, stack):
        self.nc = nc
        self.ops = []
        self.sems = {e: stack.enter_context(nc.semaphore("s_" + e)) for e in ENGS}
        self.dsems = {q: [stack.enter_context(nc.semaphore("d_%s_%d" % (q, r))) for r in range(NDMASEM)]
                      for q in ("sp", "act", "pool")}
        self.cnt = {e: 0 for e in ENGS}
        self.dcnt = {q: 0 for q in self.dsems}
        self.waited_d = {e: {} for e in ENGS}
        self.nops = 0

    def op(self, eng, fn, reads=(), writes=(), dma=False, big=False):
        self.ops.append(dict(eng=eng, fn=fn, reads=tuple(reads), writes=tuple(writes), dma=dma, big=big))

    def dma(self, q, out, in_, reads=(), writes=()):
        self.op(q, lambda e: e.dma_start(out=out, in_=in_), reads, writes, dma=True)

    def emit(self):
        nc = self.nc
        dkeys = set()
        for o in self.ops:
            if o["dma"]:
                dkeys.update(o["writes"])
        self.op("sp", None, reads=sorted(dkeys, key=str))
        ops = self.ops
        n = len(ops)
        self.nops += n
        last_w, readers = {}, {}
        deps = [None] * n
        raw = [None] * n
        for i, o in enumerate(ops):
            d = set()
            for k in o["reads"]:
                if k in last_w:
                    d.add(last_w[k])
            raw[i] = set(d)
            for k in o["writes"]:
                if k in last_w:
                    d.add(last_w[k])
                for r in readers.get(k, ()):
                    d.add(r)
            d.discard(i)
            deps[i] = d
            for k in o["writes"]:
                last_w[k] = i
                readers[k] = []
            for k in o["reads"]:
                readers.setdefault(k, []).append(i)
        seen = {e: {f: -1 for f in ENGS} for e in ENGS}
        need = [None] * n
        signal = [False] * n
        for i, o in enumerate(ops):
            E = o["eng"]
            best, keep = {}, []
            for j in deps[i]:
                oj = ops[j]
                if oj["dma"]:
                    keep.append(j)
                    continue
                Fe = oj["eng"]
                if Fe == E:
                    if E in ("pe", "sp"):
                        continue
                if seen[E][Fe] >= j:
                    continue
                if Fe not in best or best[Fe] < j:
                    best[Fe] = j
            for Fe, j in best.items():
                keep.append(j)
                seen[E][Fe] = j
                signal[j] = True
            need[i] = sorted(keep)
        need[n - 1] = sorted(set(need[n - 1]) | {i for i in range(n) if ops[i]["dma"]})
        sigval = [0] * n
        dinfo = {}
        acts = {e: [] for e in ENGS}
        for i, o in enumerate(ops):
            E = o["eng"]
            for j in need[i]:
                oj = ops[j]
                if oj["dma"]:
                    s, v, key = dinfo[j]
                    if self.waited_d[E].get(key, 0) >= v:
                        continue
                    self.waited_d[E][key] = v
                    acts[E].append(("w", s, v))
                else:
                    acts[E].append(("w", self.sems[oj["eng"]], sigval[j]))
            if o["dma"]:
                k = self.dcnt[E]
                self.dcnt[E] += 1
                r = k % NDMASEM
                s = self.dsems[E][r]
                if k >= NDMASEM:
                    v0 = 16 * (k // NDMASEM)
                    key = (E, r)
                    if self.waited_d[E].get(key, 0) < v0:
                        acts[E].append(("w", s, v0))
                        self.waited_d[E][key] = v0
                acts[E].append(("o", o["fn"], s, 16))
                dinfo[i] = (s, 16 * (k // NDMASEM + 1), (E, r))
            elif o["fn"] is not None:
                if signal[i]:
                    self.cnt[E] += 1
                    sigval[i] = self.cnt[E]
                    acts[E].append(("o", o["fn"], self.sems[E], 1))
                else:
                    acts[E].append(("o", o["fn"], None, 0))

        def replay(E):
            def f(e):
                for a in acts[E]:
                    if a[0] == "w":
                        e.wait_ge(a[1], a[2])
                    else:
                        inst = a[1](e)
                        if a[2] is not None:
                            inst.then_inc(a[2], a[3])
            return f

        with nc.Block() as block:
            block.tensor(replay("pe"))
            block.scalar(replay("act"))
            block.vector(replay("dve"))
            block.gpsimd(replay("pool"))
            block.sync(replay("sp"))
        nc.all_engine_barrier()
        self.ops = []


class Ring:
    def __init__(self, tiles, name):
        self.tiles = tiles
        self.name = name
        self.i = 0

    def next(self):
        k = self.i % len(self.tiles)
        self.i += 1
        return self.tiles[k], "%s%d" % (self.name, k)


def mm_group(P, out, pairs, reads, wkey, start=True, stop=True):
    def fn(e):
        inst = None
        n = len(pairs)
        for i, p in enumerate(pairs):
            if len(p) == 3:
                o, l, r = p
            else:
                o = out
                l, r = p
            inst = e.matmul(o, lhsT=l, rhs=r, start=(start and i == 0), stop=(stop and i == n - 1),
                            skip_group_check=True)
        return inst
    P.op("pe", fn, reads=reads, writes=[wkey])


def build_program(debug=False):
    nc = bass.Bass("TRN2", target_bir_lowering=False)

    def din(name, shape, dt=F32):
        return nc.dram_tensor(name, list(shape), dt, kind="ExternalInput").ap()

    xT = din("xT", [D, S])
    xTq = din("xTq", [D, NOWN])
    xq = din("xq", [NOWN, D])
    wKA = din("wKA", [D, 1152])
    wKB = din("wKB", [D, 1024])
    wQA = din("wQA", [D, 1024])
    wWI = din("wWI", [D, 8])
    wQB = din("wQB", [D, 512])
    wG = din("wG", [D, 2048])
    wA = din("wA", [512, D])
    wB = din("wB", [512, D])
    wO = din("wO", [D, D])
    wR = din("wR", [D, NE])
    wGU = din("wGU", [NE, D, 2048])
    wDN = din("wDN", [NE, D, D])
    bGU = din("bGU", [128, NE * 16])
    bDN = din("bDN", [NE, D])
    bRT = din("bRT", [128, NE])
    lnp = din("lnp", [4, 128, D])
    rb31 = din("rb31", [128, 8])
    t5tab = din("t5tab", [2, 8, 9, 128, TQ], BF16)
    sbmask = din("sbmask", [2, 8, 128, TQ], BF16)
    cmask = din("cmask", [2, 4, 128, 1024], BF16)
    cst_b = din("cst_b", [128, 5 * 128], BF16)
    cst_f = din("cst_f", [128, 128 + NIT + 1 + CAP])
    out = nc.dram_tensor("out", [NOWN, D], F32, kind="ExternalOutput").ap()
    dk = dict(kind="ExternalOutput") if debug else {}
    ya_d = nc.dram_tensor("ya_d", [512, NOWN], BF16, **dk).ap()
    yb_d = nc.dram_tensor("yb_d", [512, NOWN], BF16, **dk).ap()
    h1_d = nc.dram_tensor("h1_d", [NOWN, D], F32, **dk).ap()
    if debug:
        dbg_f = nc.dram_tensor("dbg_f", [128, NBLK, D], F32, kind="ExternalOutput").ap()
        dbg_g = nc.dram_tensor("dbg_g", [128, NBLK, NE], F32, kind="ExternalOutput").ap()
        dbg_p = nc.dram_tensor("dbg_p", [128, NBLK, NE], F32, kind="ExternalOutput").ap()
        dbg_x = nc.dram_tensor("dbg_x", [128, 8, CAP], BF16, kind="ExternalOutput").ap()
        dbg_a = nc.dram_tensor("dbg_a", [128, 8, CAP], BF16, kind="ExternalOutput").ap()
        dbg_y = nc.dram_tensor("dbg_y", [128, 3, D], BF16, kind="ExternalOutput").ap()

    top = ExitStack()
    with top:
        P = Prog(nc, top)

        def sb(st, name, shape, dt):
            return st.enter_context(nc.sbuf_tensor(name, list(shape), dt))

        def ps(st, name, shape=(128, 512), dt=F32):
            return st.enter_context(nc.psum_tensor(name, list(shape), dt))

        cb = sb(top, "cb", [128, 5 * 128], BF16)
        cf = sb(top, "cf", [128, 128 + NIT + 1 + CAP], F32)
        ident, negU, negones, ustrict, ones = [cb[:, i * 128:(i + 1) * 128] for i in range(5)]
        identf = cf[:, 0:128]
        pow2 = cf[:, 128:128 + NIT + 1]
        iota = cf[:, 128 + NIT + 1:128 + NIT + 1 + CAP]
        gates_all = sb(top, "gates_all", [128, NBLK, NE], F32)
        mask_b = sb(top, "mask_b", [128, NBLK, NE], BF16)
        mask_f = sb(top, "mask_f", [128, NBLK, NE], F32)
        rb31s = sb(top, "rb31s", [128, 8], F32)
        P.dma("sp", cb[:], cst_b[:, :], writes=["cb"])
        P.dma("sp", cf[:], cst_f[:, :], writes=["cf"])
        P.dma("sp", rb31s[:], rb31[:, :], writes=["rb31s"])
        psb = [ps(top, "psb%d" % i) for i in range(8)]

        def kside(st, wsrc, ncols_fm, fm_dst, tm_dst, tag):
            ncol = ncols_fm + 512
            w = sb(st, "wk" + tag, [128, 8, ncol], BF16)
            P.dma("pool", w[:], wsrc.rearrange("(c p) n -> p c n", p=128), writes=["wk"])
            xr = Ring([sb(st, "xk%s%d" % (tag, i), [128, 8, 512], BF16) for i in range(2)], "xk")
            pr = Ring(psb[0:4], "psb")
            pr.i = 0
            xTv = xT.rearrange("(c p) t -> p c t", p=128)
            for tc in range(8):
                xt, xk = xr.next()
                P.dma("pool", xt[:], xTv[:, :, tc * 512:(tc + 1) * 512], writes=[xk])
                for oc in range(ncols_fm // 128):
                    pt, pk = next_ps(pr)
                    mm_group(P, pt[:], [(w[:, dc, oc * 128:(oc + 1) * 128], xt[:, dc, :]) for dc in range(8)],
                             ["wk", xk], pk)
                    dst, dk = fm_dst(oc, tc)
                    P.op("act", lambda e, d=dst, p=pt: e.activation(out=d, in_=p[:], func=AF.Copy),
                         reads=[pk], writes=[dk])
                for tb in range(4):
                    pt, pk = next_ps(pr)
                    mm_group(P, pt[:], [(xt[:, dc, tb * 128:(tb + 1) * 128], w[:, dc, ncols_fm:ncol]) for dc in range(8)],
                             ["wk", xk], pk)
                    dst, dk = tm_dst(tc * 4 + tb)
                    P.op("dve", lambda e, d=dst, p=pt: e.tensor_copy(out=d, in_=p[:]), reads=[pk], writes=[dk])

        def next_ps(pr):
            t, k = pr.next()
            return t, k

        xTqv = xTq.rearrange("(c p) t -> p c t", p=128)

        with ExitStack() as st:
            kaT = sb(st, "kaT", [128, 4, S], BF16)
            kiT = sb(st, "kiT", [128, S], BF16)
            va = sb(st, "va", [128, 32, 512], BF16)
            with ExitStack() as st0:
                kside(st0, wKA, 640,
                      lambda oc, tc: ((kaT[:, oc, tc * 512:(tc + 1) * 512], "kaT") if oc < 4
                                      else (kiT[:, tc * 512:(tc + 1) * 512], "kiT")),
                      lambda blk: (va[:, blk, :], "va"), "A")
                P.emit()
            wq = sb(st, "wqa", [128, 8, 1024], BF16)
            wwi = sb(st, "wwi", [128, 8, 8], BF16)
            P.dma("pool", wq[:], wQA.rearrange("(c p) n -> p c n", p=128), writes=["wq"])
            P.dma("pool", wwi[:], wWI.rearrange("(c p) n -> p c n", p=128), writes=["wwi"])
            xqb = sb(st, "xqb", [128, 8, TQ], BF16)
            qaT = sb(st, "qaT", [128, 4, TQ], BF16)
            qiT = sb(st, "qiT", [128, 4, TQ], BF16)
            wis = sb(st, "wis", [128, 4, 8], F32)
            diagr = Ring([sb(st, "diag%d" % i, [128, 8, 128], BF16) for i in range(2)], "diag")
            Rr = Ring([sb(st, "R%d" % i, [128, 512], BF16) for i in range(4)], "R")
            Isbr = Ring([sb(st, "Isb%d" % i, [128, S], F32) for i in range(2)], "Isb")
            junk = sb(st, "junk", [128, S], U8)
            mbias = [sb(st, "mbias%d" % i, [128, S], BF16) for i in range(4)]
            cmr = Ring([sb(st, "cm%d" % i, [128, 1024], BF16) for i in range(2)], "cm")
            bis = sb(st, "bis", [128, 8 + NIT + 1], F32)
            bisa = sb(st, "bisa", [128, 8 + NIT + 1], F32)
            t5r = Ring([sb(st, "t5_%d" % i, [128, TQ], BF16) for i in range(4)], "t5")
            pr_ = Ring([sb(st, "p%d" % i, [128, TQ], BF16) for i in range(3)], "p")
            rden = sb(st, "rden", [64, TQ], F32)
            yor = Ring([sb(st, "yo%d" % i, [64, TQ], BF16) for i in range(2)], "yo")
            zar = Ring(psb[0:3], "psb")
            Yr = Ring(psb[4:6], "psY")
            Dr = Ring(psb[6:8], "psD")

            class _IR:
                i = 0

                def next(self):
                    self.i += 1
                    return (psb[3], "psb3") if self.i % 2 else (psb[7], "psD1")
            Ipr = _IR()
            for lg in range(NLG):
                par = lg % 2
                nkb = 8 * (lg + 1)
                Slg = 128 * nkb
                P.dma("pool", xqb[:], xTqv[:, :, lg * TQ:(lg + 1) * TQ], writes=["xqb"])
                for oc in range(8):
                    pt, pk = zar.next()
                    mm_group(P, pt[:], [(wq[:, dc, oc * 128:(oc + 1) * 128], xqb[:, dc, :]) for dc in range(8)],
                             ["wq", "xqb"], pk)
                    if oc < 4:
                        P.op("act", lambda e, d=qaT[:, oc, :], p=pt: e.mul(d, p[:], 0.125),
                             reads=[pk], writes=["qaT"])
                    else:
                        P.op("act", lambda e, d=qiT[:, oc - 4, :], p=pt: e.activation(out=d, in_=p[:], func=AF.Copy),
                             reads=[pk], writes=["qiT"])
                pt, pk = zar.next()
                for tb in range(4):
                    mm_group(P, pt[:, tb * 8:(tb + 1) * 8],
                             [(xqb[:, dc, tb * 128:(tb + 1) * 128], wwi[:, dc, :]) for dc in range(8)],
                             ["wwi", "xqb"], pk)
                P.op("dve", lambda e, p=pt: e.tensor_copy(out=wis[:].rearrange("p a b -> p (a b)"), in_=p[:, 0:32]),
                     reads=[pk], writes=["wis"])
                Istate = {}

                def IDX(tb, lg=lg, par=par, nkb=nkb):
                    dg, dgk = diagr.next()
                    for h in range(8):
                        P.op("pool", lambda e, d=dg[:, h, :], s=wis[:, tb, h:h + 1]: e.tensor_scalar(
                            out=d, in0=ident, scalar1=s, scalar2=None, op0=ALU.mult),
                            reads=["wis", "cb"], writes=[dgk])
                    cm, cmk = cmr.next()
                    P.dma("sp", cm[:], cmask[par, tb], writes=[cmk])
                    Isb, Isbk = Isbr.next()
                    nsc = nkb // 4
                    U = nsc * 8
                    units = [dict(sc=u // 8, h=u % 8) for u in range(U)]
                    Ist = {}

                    def Zf(u):
                        Uu = units[u]
                        h, sc = Uu["h"], Uu["sc"]
                        hp, hc = (h % 2) * 64, h // 2
                        Uu["zt"], Uu["zk"] = zar.next()
                        mm_group(P, Uu["zt"][:], [(qiT[hp:hp + 64, hc, tb * 128:(tb + 1) * 128],
                                                   kiT[hp:hp + 64, sc * 512:(sc + 1) * 512])], ["qiT", "kiT"], Uu["zk"])

                    def Rf(u):
                        Uu = units[u]
                        Uu["rt"], Uu["rk"] = Rr.next()
                        P.op("act", lambda e, d=Uu["rt"], p=Uu["zt"]: e.activation(out=d[:], in_=p[:], func=AF.Relu),
                             reads=[Uu["zk"]], writes=[Uu["rk"]])

                    def Df(u):
                        Uu = units[u]
                        h, sc = Uu["h"], Uu["sc"]
                        if h == 0:
                            Ist[sc] = Ipr.next()
                        Ips, Ik = Ist[sc]
                        mm_group(P, Ips[:], [(dg[:, h, :], Uu["rt"][:])], [dgk, Uu["rk"]], Ik, start=(h == 0), stop=(h == 7))
                        if h == 7:
                            dst = Isb[:, sc * 512:(sc + 1) * 512]
                            if sc >= 2 * lg:
                                c0 = (sc - 2 * lg) * 512
                                P.op("dve", lambda e, d=dst, c=cm[:, c0:c0 + 512], Ips=Ips: e.tensor_tensor(out=d, in0=Ips[:], in1=c, op=ALU.add),
                                     reads=[Ik, cmk], writes=[Isbk])
                            else:
                                P.op("dve", lambda e, d=dst, Ips=Ips: e.tensor_copy(out=d, in_=Ips[:]), reads=[Ik], writes=[Isbk])

                    for u in range(-2, U):
                        if 0 <= u + 2 < U:
                            Zf(u + 2)
                        if 0 <= u + 1 < U:
                            Rf(u + 1)
                        if 0 <= u < U:
                            Df(u)
                    Istate[tb] = (Isb, Isbk)

                def BIS(tb, which, Slg=Slg):
                    Isb, Isbk = Istate[tb]
                    Iv = Isb[:, 0:Slg]
                    mbk = "mbias%d" % tb
                    if which == 0:
                        bs, pfx, jv, jk = bis, "b", junk[:, 0:Slg], "junk"
                    else:
                        bs, pfx, jv, jk = bisa, "a", mbias[tb][:, 0:Slg], mbk
                    K = lambda n: pfx + n
                    L = []
                    A = lambda fn, reads, writes: L.append(lambda: P.op("dve", fn, reads=reads, writes=writes))
                    A(lambda e: e.reduce_max(out=bs[:, 0:1], in_=Iv, axis=mybir.AxisListType.X), [Isbk], [K("B")])
                    A(lambda e: e.tensor_scalar(out=bs[:, 5:6], in0=bs[:, 0:1], scalar1=-1.0, scalar2=None, op0=ALU.mult), [K("B")], [K("N")])
                    A(lambda e: e.tensor_tensor(out=bs[:, 6:7], in0=bs[:, 0:1], in1=bs[:, 5:6], op=ALU.max), [K("B"), K("N")], [K("A")])
                    A(lambda e: e.tensor_scalar(out=bs[:, 7:8], in0=bs[:, 6:7], scalar1=1.0, scalar2=2.0, op0=ALU.max, op1=ALU.mult), [K("A")], [K("R")])
                    A(lambda e: e.tensor_scalar(out=bs[:, 8:8 + NIT + 1], in0=pow2, scalar1=bs[:, 7:8], scalar2=None, op0=ALU.mult), [K("R"), "cf"], [K("steps")])
                    A(lambda e: e.memset(bs[:, 1:2], 0.0), [], [K("cand")])
                    for k in range(NIT):
                        A(lambda e: e.tensor_scalar(out=jv, in0=Iv, scalar1=bs[:, 1:2], scalar2=None, op0=ALU.is_ge, op1=ALU.add, accum_out=bs[:, 2:3]),
                          [Isbk, K("cand")], [K("cnt"), jk])
                        A(lambda e, k=k: e.scalar_tensor_tensor(out=bs[:, 3:4], in0=bs[:, 2:3], scalar=256.0, in1=bs[:, 8 + k:9 + k], op0=ALU.is_ge, op1=ALU.mult),
                          [K("cnt"), K("steps")], [K("inc")])
                        A(lambda e, k=k: e.scalar_tensor_tensor(out=bs[:, 1:2], in0=bs[:, 3:4], scalar=bs[:, 9 + k:10 + k], in1=bs[:, 1:2], op0=ALU.subtract, op1=ALU.add),
                          [K("inc"), K("steps"), K("cand")], [K("cand")])
                    A(lambda e: e.tensor_tensor(out=bs[:, 4:5], in0=bs[:, 1:2], in1=bs[:, 8 + NIT:9 + NIT], op=ALU.subtract), [K("cand"), K("steps")], [K("thr")])
                    A(lambda e: e.tensor_scalar(out=bs[:, 5:6], in0=bs[:, 7:8], scalar1=1.0 - 2.0 ** -(NIT + 1), scalar2=None, op0=ALU.mult), [K("R"), K("A")], [K("M")])
                    A(lambda e: e.tensor_tensor(out=bs[:, 6:7], in0=bs[:, 4:5], in1=bs[:, 5:6], op=ALU.add), [K("thr"), K("M"), K("A")], [K("T1")])
                    A(lambda e: e.tensor_scalar(out=bs[:, 6:7], in0=bs[:, 6:7], scalar1=0.0, scalar2=-1e29, op0=ALU.is_le, op1=ALU.mult), [K("T1")], [K("Pen")])
                    A(lambda e: e.tensor_tensor(out=bs[:, 3:4], in0=bs[:, 4:5], in1=bs[:, 6:7], op=ALU.add), [K("thr"), K("Pen"), K("inc")], [K("thr2")])
                    A(lambda e, d=mbias[tb][:, 0:Slg]: e.tensor_scalar(out=d, in0=Iv, scalar1=bs[:, 3:4], scalar2=NEG, op0=ALU.is_lt, op1=ALU.mult),
                      [Isbk, K("thr2")], [mbk])
                    return L

                for t0 in (0, 2):
                    IDX(t0)
                    IDX(t0 + 1)
                    La, Lb = BIS(t0, 0), BIS(t0 + 1, 1)
                    for fa, fb in zip(La, Lb):
                        fa()
                        fb()
                blocks = [dict(h=h, kb=kb) for h in range(8) for kb in range(nkb)]
                NB = len(blocks)
                YD = {}

                def Af(b):
                    B = blocks[b]
                    h, kb = B["h"], B["kb"]
                    hp, hc = (h % 2) * 64, h // 2
                    near = kb >= 8 * lg - 1
                    B["near"] = near
                    B["at"], B["ak"] = zar.next()
                    at = B["at"]
                    pairs = [(at[:], kaT[hp:hp + 64, hc, kb * 128:(kb + 1) * 128], qaT[hp:hp + 64, hc, :])]
                    rd = ["kaT", "qaT", "cb"] + ["mbias%d" % t for t in range(4)]
                    for tb in range(4):
                        pairs.append((at[:, tb * 128:(tb + 1) * 128], mbias[tb][:, kb * 128:(kb + 1) * 128], ident))
                    if near:
                        t5, t5k = t5r.next()
                        P.dma("sp", t5[:], t5tab[par, h, kb - 8 * lg + 1], writes=[t5k])
                        pairs.append((at[:], ident, t5[:]))
                        rd.append(t5k)
                    mm_group(P, at[:], pairs, rd, B["ak"])

                def Xf(b):
                    B = blocks[b]
                    h = B["h"]
                    B["pt"], B["pk"] = pr_.next()
                    if B["near"]:
                        P.op("act", lambda e, d=B["pt"], a=B["at"]: e.activation(out=d[:], in_=a[:], func=AF.Exp),
                             reads=[B["ak"]], writes=[B["pk"]])
                    else:
                        P.op("act", lambda e, d=B["pt"], a=B["at"], b_=rb31s[:, h:h + 1]: e.activation(out=d[:], in_=a[:], func=AF.Exp, bias=b_),
                             reads=[B["ak"], "rb31s"], writes=[B["pk"]])

                def Vf(b):
                    B = blocks[b]
                    h, kb = B["h"], B["kb"]
                    if kb == 0:
                        YD[h] = (Yr.next(), Dr.next())
                    (Yt, Yk), (Dt, Dk) = YD[h]
                    mm_group(P, Yt[0:64, :], [(va[:, kb, h * 64:(h + 1) * 64], B["pt"][:])], ["va", B["pk"]], Yk,
                             start=(kb == 0), stop=(kb == nkb - 1))
                    mm_group(P, Dt[0:64, :], [(ones[:, 0:64], B["pt"][:])], ["cb", B["pk"]], Dk,
                             start=(kb == 0), stop=(kb == nkb - 1))
                    if kb == nkb - 1:
                        P.op("dve", lambda e, d=Dt: e.reciprocal(out=rden[:], in_=d[0:64, :]), reads=[Dk], writes=["rden"])
                        yo, yok = yor.next()
                        P.op("dve", lambda e, y=Yt, o=yo: e.tensor_tensor(out=o[:], in0=y[0:64, :], in1=rden[:], op=ALU.mult),
                             reads=[Yk, "rden"], writes=[yok])
                        P.dma("sp", ya_d[h * 64:(h + 1) * 64, lg * TQ:(lg + 1) * TQ], yo[:], reads=[yok], writes=["ya_d%d_%d" % (lg, h)])

                for i in range(-2, NB):
                    if 0 <= i + 2 < NB:
                        Af(i + 2)
                    if 0 <= i + 1 < NB:
                        Xf(i + 1)
                    if 0 <= i < NB:
                        Vf(i)
            P.emit()

        with ExitStack() as st:
            kbT = sb(st, "kbT", [128, 4, S], BF16)
            vb = sb(st, "vb", [128, 32, 512], BF16)
            with ExitStack() as st0:
                kside(st0, wKB, 512, lambda oc, tc: (kbT[:, oc, tc * 512:(tc + 1) * 512], "kbT"),
                      lambda blk: (vb[:, blk, :], "vb"), "B")
                P.emit()
            wq = sb(st, "wqb", [128, 8, 512], BF16)
            P.dma("pool", wq[:], wQB.rearrange("(c p) n -> p c n", p=128), writes=["wq"])
            xqb = sb(st, "xqb2", [128, 8, TQ], BF16)
            qbT = sb(st, "qbT", [128, 4, TQ], BF16)
            sbm = sb(st, "sbm", [128, 8, TQ], BF16)
            er = Ring([sb(st, "e%d" % i, [128, TQ], F32) for i in range(2)], "e")
            spr = Ring([sb(st, "sp%d" % i, [128, TQ], BF16) for i in range(3)], "spl")
            wr_ = Ring([sb(st, "w%d" % i, [128, TQ], BF16) for i in range(3)], "w")
            accr = Ring([sb(st, "acc%d" % i, [128, TQ], BF16) for i in range(2)], "acc")
            yor = Ring([sb(st, "yob%d" % i, [64, TQ], BF16) for i in range(2)], "yob")
            ar = Ring(psb[0:4], "psb")
            Yr = Ring(psb[4:6], "psY")
            for lg in range(NLG):
                par = lg % 2
                nkb = 8 * (lg + 1)
                P.dma("pool", xqb[:], xTqv[:, :, lg * TQ:(lg + 1) * TQ], writes=["xqb"])
                P.dma("sp", sbm[:], sbmask[par].rearrange("j p t -> p j t"), writes=["sbm"])
                for oc in range(4):
                    pt, pk = ar.next()
                    mm_group(P, pt[:], [(wq[:, dc, oc * 128:(oc + 1) * 128], xqb[:, dc, :]) for dc in range(8)],
                             ["wq", "xqb"], pk)
                    P.op("act", lambda e, d=qbT[:, oc, :], p=pt: e.mul(d, p[:], 0.125),
                         reads=[pk], writes=["qbT"])
                blocks = []
                for h in range(8):
                    for idx, kb in enumerate(range(nkb - 1, -1, -1)):
                        blocks.append(dict(h=h, idx=idx, kb=kb, first=(idx == 0), last=(idx == nkb - 1)))
                NB = len(blocks)
                Ystate = {}

                def S1(b):
                    B = blocks[b]
                    h, kb = B["h"], B["kb"]
                    hp, hc = (h % 2) * 64, h // 2
                    B["at"], B["ak"] = ar.next()
                    pairs = [(kbT[hp:hp + 64, hc, kb * 128:(kb + 1) * 128], qbT[hp:hp + 64, hc, :])]
                    rd = ["kbT", "qbT"]
                    if kb >= 8 * lg:
                        pairs.append((ident, sbm[:, kb - 8 * lg, :]))
                        rd += ["cb", "sbm"]
                    mm_group(P, B["at"][:], pairs, rd, B["ak"], stop=False)

                def E1L(b):
                    B = blocks[b]
                    et, ek = er.next()
                    P.op("act", lambda e, d=et, a=B["at"]: e.activation(out=d[:], in_=a[:], func=AF.Exp), reads=[B["ak"]], writes=[ek], big=True)
                    B["sp"], B["spk"] = spr.next()
                    P.op("act", lambda e, d=B["sp"], a=et: e.activation(out=d[:], in_=a[:], func=AF.Ln, bias=1.0, scale=1.0),
                         reads=[ek], writes=[B["spk"]], big=True)
                    if not B["last"]:
                        acn, acnk = accr.next()
                        if B["first"]:
                            P.op("pool", lambda e, d=acn, s_=B["sp"]: e.tensor_copy(out=d[:], in_=s_[:]), reads=[B["spk"]], writes=[acnk])
                        else:
                            pa_, pak_ = B["acc"]
                            P.op("pool", lambda e, d=acn, s_=B["sp"], a=pa_: e.tensor_tensor(out=d[:], in0=a[:], in1=s_[:], op=ALU.add),
                                 reads=[B["spk"], pak_], writes=[acnk])
                        blocks[b + 1]["acc"] = (acn, acnk)

                def S2(b):
                    B = blocks[b]
                    pairs = [(negU, B["sp"][:])]
                    rd = ["cb", B["spk"]]
                    if not B["first"]:
                        pairs.append((negones, B["acc"][0][:]))
                        rd.append(B["acc"][1])
                    mm_group(P, B["at"][:], pairs, rd, B["ak"], start=False)

                def E2(b):
                    B = blocks[b]
                    B["w"], B["wk"] = wr_.next()
                    P.op("act", lambda e, d=B["w"], a=B["at"]: e.activation(out=d[:], in_=a[:], func=AF.Exp), reads=[B["ak"]], writes=[B["wk"]], big=True)

                def S3(b):
                    B = blocks[b]
                    h, kb = B["h"], B["kb"]
                    if B["first"]:
                        Ystate[h] = Yr.next()
                    Yt, Yk = Ystate[h]
                    mm_group(P, Yt[0:64, :], [(vb[:, kb, h * 64:(h + 1) * 64], B["w"][:])], ["vb", B["wk"]], Yk,
                             start=B["first"], stop=B["last"])
                    if B["last"]:
                        yo, yok = yor.next()
                        P.op("dve", lambda e, y=Yt, o=yo: e.tensor_copy(out=o[:], in_=y[0:64, :]), reads=[Yk], writes=[yok])
                        P.dma("sp", yb_d[h * 64:(h + 1) * 64, lg * TQ:(lg + 1) * TQ], yo[:], reads=[yok], writes=["yb_d%d_%d" % (lg, h)])

                for i in range(-2, NB + 1):
                    if 0 <= i < NB:
                        S2(i)
                    if 0 <= i + 2 < NB:
                        S1(i + 2)
                    if 0 <= i - 1 < NB:
                        S3(i - 1)
                    if 0 <= i + 1 < NB:
                        E1L(i + 1)
                    if 0 <= i < NB:
                        E2(i)
            P.emit()

        with ExitStack() as st:
            wg = sb(st, "wg", [128, 8, 2048], BF16)
            wa = sb(st, "wa", [128, 4, D], BF16)
            wb = sb(st, "wb", [128, 4, D], BF16)
            wo = sb(st, "wo", [128, 8, D], BF16)
            wr = sb(st, "wr", [128, 8, NE], F32)
            brt = sb(st, "brt", [128, NE], F32)
            lnps = sb(st, "lnps", [128, 2, D], F32)
            P.dma("pool", wg[:], wG.rearrange("(c p) n -> p c n", p=128), writes=["wg"])
            P.dma("pool", wa[:], wA.rearrange("(c p) n -> p c n", p=128), writes=["wa"])
            P.dma("pool", wb[:], wB.rearrange("(c p) n -> p c n", p=128), writes=["wb"])
            P.dma("pool", wo[:], wO.rearrange("(c p) n -> p c n", p=128), writes=["wo"])
            P.dma("sp", wr[:], wR.rearrange("(c p) n -> p c n", p=128), writes=["wr"])
            P.dma("sp", brt[:], bRT[:, :], writes=["brt"])
            P.dma("sp", lnps[:], lnp[0:2].rearrange("a p d -> p a d"), writes=["lnps"])
            xqb = sb(st, "xqb3", [128, 8, TQ], BF16)
            yas = sb(st, "yas", [128, 4, TQ], BF16)
            ybs = sb(st, "ybs", [128, 4, TQ], BF16)
            sg = sb(st, "sg", [128, 16, TQ], BF16)
            mT = sb(st, "mT", [128, 8, TQ], BF16)
            t1 = sb(st, "t1", [128, TQ], F32)
            t2 = sb(st, "t2", [128, TQ], F32)
            xres = sb(st, "xres", [128, D], F32)
            r1 = sb(st, "r1", [128, D], F32)
            sq = sb(st, "sq", [128, D], F32)
            h1t = sb(st, "h1t", [128, D], F32)
            h1T = sb(st, "h1T", [128, 8, 128], F32)
            sm = sb(st, "sm", [128, 64], F32)
            pr = Ring(psb, "psb")
            for lg in range(NLG):
                P.dma("pool", xqb[:], xTqv[:, :, lg * TQ:(lg + 1) * TQ], writes=["xqb"])
                P.dma("sp", yas[:], ya_d[:, lg * TQ:(lg + 1) * TQ].rearrange("(c p) t -> p c t", p=128), writes=["yas"])
                P.dma("sp", ybs[:], yb_d[:, lg * TQ:(lg + 1) * TQ].rearrange("(c p) t -> p c t", p=128), writes=["ybs"])
                for oc in range(16):
                    pt, pk = pr.next()
                    mm_group(P, pt[:], [(wg[:, dc, oc * 128:(oc + 1) * 128], xqb[:, dc, :]) for dc in range(8)], ["wg", "xqb"], pk)
                    P.op("act", lambda e, d=sg[:, oc, :], p=pt: e.activation(out=d, in_=p[:], func=AF.Sigmoid), reads=[pk], writes=["sg"])
                for oc in range(8):
                    pa, pak = pr.next()
                    mm_group(P, pa[:], [(wa[:, fc, oc * 128:(oc + 1) * 128], yas[:, fc, :]) for fc in range(4)], ["wa", "yas"], pak)
                    pb, pbk = pr.next()
                    mm_group(P, pb[:], [(wb[:, fc, oc * 128:(oc + 1) * 128], ybs[:, fc, :]) for fc in range(4)], ["wb", "ybs"], pbk)
                    P.op("dve", lambda e, p=pa, g=sg[:, oc, :]: e.tensor_tensor(out=t1[:], in0=p[:], in1=g, op=ALU.mult), reads=[pak, "sg"], writes=["t1"])
                    P.op("dve", lambda e, p=pb, g=sg[:, 8 + oc, :]: e.tensor_tensor(out=t2[:], in0=p[:], in1=g, op=ALU.mult), reads=[pbk, "sg"], writes=["t2"])
                    P.op("pool", lambda e, d=mT[:, oc, :]: e.tensor_tensor(out=d, in0=t1[:], in1=t2[:], op=ALU.add), reads=["t1", "t2"], writes=["mT"])
                for tb in range(4):
                    blk = lg * 4 + tb
                    P.dma("sp", xres[:], xq[blk * 128:(blk + 1) * 128, :], writes=["xres"])
                    for half in range(2):
                        pm, pmk = pr.next()
                        mm_group(P, pm[:], [(mT[:, dc, tb * 128:(tb + 1) * 128], wo[:, dc, half * 512:(half + 1) * 512]) for dc in range(8)],
                                 ["mT", "wo"], pmk)
                        P.op("dve", lambda e, p=pm, hs=slice(half * 512, (half + 1) * 512): e.scalar_tensor_tensor(
                            out=r1[:, hs], in0=xres[:, hs], scalar=ALPHA, in1=p[:], op0=ALU.mult, op1=ALU.add),
                            reads=[pmk, "xres"], writes=["r1"])
                    layer_norm(P, r1, sq, sm, lnps, h1t, "r1", "h1t")
                    P.dma("sp", h1_d[blk * 128:(blk + 1) * 128, :], h1t[:], reads=["h1t"], writes=["h1_d%d" % blk])
                    for half in range(2):
                        ptt, ptk = pr.next()
                        mm_group(P, ptt[:], [(ptt[:, j * 128:(j + 1) * 128], h1t[:, (half * 4 + j) * 128:(half * 4 + j + 1) * 128], identf)
                                             for j in range(4)], ["h1t", "cf"], ptk)
                        P.op("act", lambda e, p=ptt, d=h1T[:, half * 4:(half + 1) * 4, :]: e.activation(
                            out=d.rearrange("p a b -> p (a b)"), in_=p[:], func=AF.Copy), reads=[ptk], writes=["h1T"])
                    prr, prk = pr.next()
                    mm_group(P, prr[:, 0:NE], [(h1T[:, dc, :], wr[:, dc, :]) for dc in range(8)], ["h1T", "wr"], prk)

                    lgt = sm[:, 16:48]
                    P.op("dve", lambda e, p=prr: e.tensor_tensor(out=sm[:, 16:48], in0=p[:, 0:NE], in1=brt[:], op=ALU.add),
                         reads=[prk, "brt"], writes=["rlg"])
                    P.op("dve", lambda e: e.max(out=sm[:, 0:8], in_=sm[:, 16:48]), reads=["rlg"], writes=["rtop"])
                    P.op("dve", lambda e, blk=blk: e.tensor_scalar(out=mask_f[:, blk, :], in0=sm[:, 16:48], scalar1=sm[:, 3:4], scalar2=None, op0=ALU.is_ge),
                         reads=["rlg", "rtop"], writes=["maskf"])
                    P.op("dve", lambda e, blk=blk: e.tensor_copy(out=mask_b[:, blk, :], in_=mask_f[:, blk, :]), reads=["maskf"], writes=["maskb"])
                    P.op("dve", lambda e: e.tensor_scalar(out=sm[:, 48:49], in0=sm[:, 0:1], scalar1=-1.0, scalar2=None, op0=ALU.mult),
                         reads=["rtop"], writes=["rneg"])
                    P.op("act", lambda e: e.activation(out=sm[:, 16:48], in_=sm[:, 16:48], func=AF.Exp, bias=sm[:, 48:49]),
                         reads=["rlg", "rneg", "maskf"], writes=["rex"])
                    P.op("dve", lambda e, blk=blk: e.tensor_tensor(out=sm[:, 16:48], in0=sm[:, 16:48], in1=mask_f[:, blk, :], op=ALU.mult),
                         reads=["rex", "maskf"], writes=["rexm"])
                    P.op("dve", lambda e: e.reduce_sum(out=sm[:, 8:9], in_=sm[:, 16:48], axis=mybir.AxisListType.X), reads=["rexm"], writes=["rden"])
                    P.op("dve", lambda e: e.reciprocal(out=sm[:, 9:10], in_=sm[:, 8:9]), reads=["rden"], writes=["rrd"])
                    P.op("dve", lambda e, blk=blk: e.tensor_scalar(out=gates_all[:, blk, :], in0=sm[:, 16:48], scalar1=sm[:, 9:10], scalar2=None, op0=ALU.mult),
                         reads=["rexm", "rrd"], writes=["gates", "rlg"])
            P.emit()

        with ExitStack() as stf:
          f = sb(stf, "f", [128, NBLK, D], F32)
          with ExitStack() as st:
            h1b = sb(st, "h1b", [128, NBLK, D], BF16)
            P.dma("pool", h1b[:], h1_d.rearrange("(b p) d -> p b d", p=128), reads=["h1_d"], writes=["h1b"])
            P.op("pool", lambda e: e.memset(f[:], 0.0), writes=["f%d" % b for b in range(NBLK)])
            wgur = Ring([sb(st, "wgu%d" % i, [128, 8, D], BF16) for i in range(2)], "wgu")
            wdnr = Ring([sb(st, "wdn%d" % i, [128, 4, D], BF16) for i in range(2)], "wdn")
            bgu = sb(st, "bgu", [128, NE * 16], F32)
            P.dma("sp", bgu[:], bGU[:, :], writes=["bgu"])
            posm = sb(st, "posm", [128, NBLK, NE], F32)
            Se = sb(st, "Se", [128, NBLK, CAP], BF16)
            STe = sb(st, "STe", [128, 3, NOWN], BF16)
            xg = sb(st, "xg", [128, 8, CAP], BF16)
            actT = sb(st, "actT", [128, 8, CAP], BF16)
            Ysb = sb(st, "Ysb", [128, 3, D], BF16)
            tar = Ring([sb(st, "ta%d" % i, [128, CAP], F32) for i in range(2)], "ta")
            tsr = Ring([sb(st, "tsg%d" % i, [128, CAP], F32) for i in range(2)], "tsg")
            tur = Ring([sb(st, "tu%d" % i, [128, CAP], F32) for i in range(2)], "tu")
            tgr = Ring([sb(st, "tg%d" % i, [128, CAP], F32) for i in range(2)], "tg")
            bgs = sb(st, "bgs", [128, NE * 16], F32)
            P.op("dve", lambda e: e.tensor_scalar(out=bgs[:], in0=bgu[:], scalar1=1.702, scalar2=None, op0=ALU.mult), reads=["bgu"], writes=["bgs"])
            pr = Ring(psb, "psb")
            for blk in range(NBLK):
                pt, pk = pr.next()
                pairs = [(ustrict, mask_b[:, blk, :])] + [(ones, mask_b[:, b2, :]) for b2 in range(blk)]
                mm_group(P, pt[:, 0:NE], pairs, ["cb", "maskb"], pk)

                P.op("dve", lambda e, p=pt, blk=blk: e.scalar_tensor_tensor(out=posm[:, blk, :], in0=p[:, 0:NE], scalar=1.0, in1=mask_f[:, blk, :],
                                                                           op0=ALU.add, op1=ALU.mult), reads=[pk, "maskf"], writes=["posm1"])
                P.op("dve", lambda e, blk=blk: e.tensor_scalar(out=posm[:, blk, :], in0=posm[:, blk, :], scalar1=-1.0, scalar2=None, op0=ALU.add),
                     reads=["posm1"], writes=["posm"])
            def build_Se(ex):
                for blk in range(NBLK):
                    P.op("dve", lambda e, d=Se[:, blk, :], s=posm[:, blk, ex:ex + 1]: e.tensor_scalar(
                        out=d, in0=iota, scalar1=s, scalar2=None, op0=ALU.is_equal), reads=["posm", "cf"], writes=["Se"])

            WG, WD = {}, {}

            def LOAD_GU(ex):
                WG[ex] = []
                for half in range(2):
                    t, k = wgur.next()
                    P.dma("pool", t[:], wGU[ex, :, half * D:(half + 1) * D].rearrange("(c p) n -> p c n", p=128), writes=[k])
                    WG[ex].append((t, k))

            def LOAD_DN(ex):
                WD[ex] = []
                for half in range(2):
                    t, k = wdnr.next()
                    P.dma("pool", t[:], wDN[ex, half * 512:(half + 1) * 512, :].rearrange("(c p) n -> p c n", p=128), writes=[k])
                    WD[ex].append((t, k))

            def ST(ex):
                for ci, (c0, cs) in enumerate(CBS):
                    for tq in range(4):
                        pt, pk = pr.next()
                        mm_group(P, pt[:], [(pt[0:cs, j * 128:(j + 1) * 128], Se[:, tq * 4 + j, c0:c0 + cs], ident) for j in range(4)],
                                 ["Se", "cb"], pk)
                        P.op("act", lambda e, p=pt, d=STe[0:cs, ci, tq * 512:(tq + 1) * 512], cs=cs: e.activation(out=d, in_=p[0:cs, :], func=AF.Copy),
                             reads=[pk], writes=["STe"])

            def GATHER(ex):
                for dc in range(8):
                    pt, pk = pr.next()
                    mm_group(P, pt[:, 0:CAP], [(h1b[:, blk, dc * 128:(dc + 1) * 128], Se[:, blk, :]) for blk in range(NBLK)],
                             ["h1b", "Se"], pk)
                    P.op("act", lambda e, p=pt, d=xg[:, dc, :]: e.activation(out=d, in_=p[:, 0:CAP], func=AF.Copy), reads=[pk], writes=["xg"])

            def GATEUP(ex):
                wa_, wak = WG[ex][0]
                wu_, wuk = WG[ex][1]
                for fc in range(8):
                    pa_, pak = pr.next()
                    mm_group(P, pa_[:, 0:CAP], [(wa_[:, dc, fc * 128:(fc + 1) * 128], xg[:, dc, :]) for dc in range(8)], [wak, "xg"], pak)
                    pu_, puk = pr.next()
                    mm_group(P, pu_[:, 0:CAP], [(wu_[:, dc, fc * 128:(fc + 1) * 128], xg[:, dc, :]) for dc in range(8)], [wuk, "xg"], puk)
                    ca = ex * 16 + fc
                    cu = ex * 16 + 8 + fc
                    ta, tak = tar.next()
                    tsg, tsk = tsr.next()
                    tu, tuk = tur.next()
                    tg, tgk = tgr.next()
                    P.op("dve", lambda e, p=pa_, b=bgu[:, ca:ca + 1], d=ta: e.tensor_scalar(out=d[:], in0=p[:, 0:CAP], scalar1=b, scalar2=7.0, op0=ALU.add, op1=ALU.min),
                         reads=[pak, "bgu"], writes=[tak])
                    P.op("act", lambda e, a=ta, d=tsg: e.activation(out=d[:], in_=a[:], func=AF.Sigmoid, scale=1.702),
                         reads=[tak], writes=[tsk])
                    P.op("dve", lambda e, p=pu_, b=bgu[:, cu:cu + 1], d=tu: e.tensor_scalar(out=d[:], in0=p[:, 0:CAP], scalar1=b, scalar2=7.0, op0=ALU.add, op1=ALU.min),
                         reads=[puk, "bgu"], writes=[tuk])
                    P.op("pool", lambda e, d=tg, a=ta, g=tsg: e.tensor_tensor(out=d[:], in0=a[:], in1=g[:], op=ALU.mult), reads=[tak, tsk], writes=[tgk])
                    P.op("dve", lambda e, d=tu: e.tensor_scalar(out=d[:], in0=d[:], scalar1=-7.0, scalar2=1.0, op0=ALU.max, op1=ALU.add),
                         reads=[tuk], writes=[tuk])
                    P.op("pool", lambda e, d=actT[:, fc, :], u=tu, g=tg: e.tensor_tensor(out=d, in0=u[:], in1=g[:], op=ALU.mult),
                         reads=[tuk, tgk], writes=["actT"])

            def DOWN(ex):
                wd_t = WD[ex]
                for ci, (c0, cs) in enumerate(CBS):
                    for half in range(2):
                        pt, pk = pr.next()
                        mm_group(P, pt[0:cs, :], [(actT[:, fc, c0:c0 + cs], wd_t[fc // 4][0][:, fc % 4, half * 512:(half + 1) * 512]) for fc in range(8)],
                                 ["actT", wd_t[0][1], wd_t[1][1]], pk)
                        P.op("act", lambda e, p=pt, d=Ysb[0:cs, ci, half * 512:(half + 1) * 512], cs=cs: e.activation(out=d, in_=p[0:cs, :], func=AF.Copy),
                             reads=[pk], writes=["Ysb"])

            def SCATTER(ex):
                for blk in range(NBLK):
                    for half in range(2):
                        pt, pk = pr.next()
                        mm_group(P, pt[:], [(STe[0:cs, ci, blk * 128:(blk + 1) * 128], Ysb[0:cs, ci, half * 512:(half + 1) * 512])
                                            for ci, (c0, cs) in enumerate(CBS)], ["STe", "Ysb"], pk)
                        fs = f[:, blk, half * 512:(half + 1) * 512]
                        P.op("dve", lambda e, p=pt, fs=fs, g=gates_all[:, blk, ex:ex + 1]: e.scalar_tensor_tensor(
                            out=fs, in0=p[:], scalar=g, in1=fs, op0=ALU.mult, op1=ALU.add),
                            reads=[pk, "gates", "f%d" % blk], writes=["f%d" % blk])

            LOAD_GU(0)
            LOAD_DN(0)
            build_Se(0)
            ST(0)
            GATHER(0)
            for ex in range(NE):
                if ex + 1 < NE:
                    build_Se(ex + 1)
                GATEUP(ex)
                if ex + 1 < NE:
                    LOAD_GU(ex + 1)
                    GATHER(ex + 1)
                DOWN(ex)
                if ex + 1 < NE:
                    LOAD_DN(ex + 1)
                SCATTER(ex)
                if ex + 1 < NE:
                    ST(ex + 1)
            if debug:
                P.dma("sp", dbg_f, f[:], reads=["f%d" % b for b in range(NBLK)], writes=["dbg_f"])
                P.dma("sp", dbg_g, gates_all[:], reads=["gates"], writes=["dbg_g"])
                P.dma("sp", dbg_p, posm[:], reads=["posm"], writes=["dbg_p"])
                P.dma("sp", dbg_x, xg[:], reads=["xg"], writes=["dbg_x"])
                P.dma("sp", dbg_a, actT[:], reads=["actT"], writes=["dbg_a"])
                P.dma("sp", dbg_y, Ysb[:], reads=["Ysb"], writes=["dbg_y"])
            P.emit()
          with ExitStack() as st:
            pr = Ring(psb, "psb")
            bdn = sb(st, "bdn", [NE, D], BF16)
            lnps = sb(st, "lnps2", [128, 2, D], F32)
            P.dma("pool", bdn[:], bDN[:, :], writes=["bdn"])
            P.dma("sp", lnps[:], lnp[2:4].rearrange("a p d -> p a d"), writes=["lnps"])
            h1r = Ring([sb(st, "h1r%d" % i, [128, D], F32) for i in range(2)], "h1r")
            r2s = [sb(st, "r2_%d" % i, [128, D], F32) for i in range(2)]
            sqs = [sb(st, "sq2_%d" % i, [128, D], F32) for i in range(2)]
            sms = [sb(st, "sm2_%d" % i, [128, 64], F32) for i in range(2)]
            gTs = [sb(st, "gT%d" % i, [NE, 128], BF16) for i in range(2)]
            outr = Ring([sb(st, "ot%d" % i, [128, D], F32) for i in range(2)], "ot")

            def final_block(Q, blk, ch):
                r2, sq, sm, gT = r2s[ch], sqs[ch], sms[ch], gTs[ch]
                sfx = "_%d" % ch
                ht, hk = h1r.next()
                Q.dma("sp", ht[:], h1_d[blk * 128:(blk + 1) * 128, :], reads=["h1_d"], writes=[hk])
                pt, pk = pr.next()
                mm_group(Q, pt[0:NE, 0:128], [(gates_all[:, blk, :], identf)], ["gates", "cf"], pk)
                Q.op("act", lambda e, p=pt: e.activation(out=gT[:], in_=p[0:NE, 0:128], func=AF.Copy), reads=[pk], writes=["gT" + sfx])
                for half in range(2):
                    hs = slice(half * 512, (half + 1) * 512)
                    pb_, pbk = pr.next()
                    mm_group(Q, pb_[:], [(gT[:], bdn[:, hs])], ["gT" + sfx, "bdn"], pbk)
                    Q.op("dve", lambda e, hs=hs, ht=ht, blk=blk: e.scalar_tensor_tensor(
                        out=r2[:, hs], in0=ht[:, hs], scalar=ALPHA, in1=f[:, blk, hs], op0=ALU.mult, op1=ALU.add),
                        reads=[hk, "f%d" % blk], writes=["r2" + sfx])
                    Q.op("dve", lambda e, hs=hs, p=pb_: e.tensor_tensor(out=r2[:, hs], in0=r2[:, hs], in1=p[:], op=ALU.add),
                         reads=[pbk, "r2" + sfx], writes=["r2" + sfx])
                ot, otk = outr.next()
                layer_norm(Q, r2, sq, sm, lnps, ot, "r2" + sfx, otk, sfx=sfx)
                Q.dma("sp", out[blk * 128:(blk + 1) * 128, :], ot[:], reads=[otk], writes=["out%d" % blk])

            for b0 in range(0, NBLK, 2):
                Qa, Qb = Deferred(P), Deferred(P)
                final_block(Qa, b0, 0)
                final_block(Qb, b0 + 1, 1)
                for fa, fb in zip(Qa.L, Qb.L):
                    fa()
                    fb()
            P.emit()
    return nc


class Deferred:
    def __init__(self, P):
        self.P = P
        self.L = []

    def op(self, *a, **k):
        self.L.append(lambda: self.P.op(*a, **k))

    def dma(self, *a, **k):
        self.L.append(lambda: self.P.dma(*a, **k))


def layer_norm(P, src, sq, sm, lnps, dst, skey, dkey, sfx=""):
    invn = 1.0 / D
    K = lambda n: n + sfx
    P.op("act", lambda e: e.activation(out=sq[:], in_=src[:], func=AF.Square), reads=[skey], writes=[K("sq")])
    P.op("dve", lambda e: e.tensor_scalar(out=dst[:], in0=src[:], scalar1=invn, scalar2=None, op0=ALU.mult, op1=ALU.add, accum_out=sm[:, 56:57]),
         reads=[skey], writes=[K("lmean"), dkey])
    P.op("dve", lambda e: e.tensor_scalar(out=dst[:], in0=sq[:], scalar1=invn, scalar2=None, op0=ALU.mult, op1=ALU.add, accum_out=sm[:, 57:58]),
         reads=[K("sq")], writes=[K("lex2"), dkey])
    P.op("dve", lambda e: e.tensor_tensor(out=sm[:, 58:59], in0=sm[:, 56:57], in1=sm[:, 56:57], op=ALU.mult), reads=[K("lmean")], writes=[K("lm2")])
    P.op("dve", lambda e: e.tensor_tensor(out=sm[:, 59:60], in0=sm[:, 57:58], in1=sm[:, 58:59], op=ALU.subtract), reads=[K("lex2"), K("lm2")], writes=[K("lvar0")])
    P.op("dve", lambda e: e.tensor_scalar(out=sm[:, 62:63], in0=sm[:, 59:60], scalar1=0.0, scalar2=LN_EPS, op0=ALU.max, op1=ALU.add),
         reads=[K("lvar0")], writes=[K("lvar")])
    P.op("act", lambda e: e.activation(out=sm[:, 60:61], in_=sm[:, 62:63], func=AF.Sqrt), reads=[K("lvar")], writes=[K("lstd")])
    P.op("dve", lambda e: e.reciprocal(out=sm[:, 61:62], in_=sm[:, 60:61]), reads=[K("lstd")], writes=[K("lrstd")])
    P.op("dve", lambda e: e.tensor_scalar(out=dst[:], in0=src[:], scalar1=sm[:, 56:57], scalar2=sm[:, 61:62], op0=ALU.subtract, op1=ALU.mult),
         reads=[K("lmean"), K("lrstd"), skey], writes=[dkey])
    P.op("dve", lambda e: e.tensor_tensor(out=dst[:], in0=dst[:], in1=lnps[:, 0, :], op=ALU.mult), reads=[dkey, "lnps"], writes=[dkey])
    P.op("dve", lambda e: e.tensor_tensor(out=dst[:], in0=dst[:], in1=lnps[:, 1, :], op=ALU.add), reads=[dkey, "lnps"], writes=[dkey])


def _t5_bucket(rel):
    n = np.maximum(rel, 0)
    nf = np.maximum(n, 1).astype(np.float32)
    large = 16 + (np.log(nf / np.float32(16)) / np.float32(math.log(128 / 16)) * np.float32(16)).astype(np.int32)
    large = np.minimum(large, 31)
    return np.where(n < 16, n, large)


def _host_tables(rel_bias, hf):
    ss = np.arange(128)[:, None]
    tt = np.arange(TQ)[None, :]
    offs = [0, 4] if hf == 0 else [4, 0]
    t5 = np.empty((2, 8, 9, 128, TQ), np.float32)
    sbm = np.empty((2, 8, 128, TQ), np.float32)
    cm = np.empty((2, 4, 128, 1024), np.float32)
    for par, off in enumerate(offs):
        for j in range(8):
            rel = 128 * (off - j) + tt - ss
            sbm[par, j] = np.where(rel > 0, 0.0, NEG)
        for jj in range(9):
            rel = 128 * (off - (jj - 1)) + tt - ss
            bk = _t5_bucket(rel)
            for h in range(8):
                t5[par, h, jj] = np.where(rel >= 0, rel_bias[bk, h], NEG)
        for tb in range(4):
            tl = np.arange(128)[:, None]
            sg = np.arange(1024)[None, :]
            rel = 128 * off + 128 * tb + tl - sg
            cm[par, tb] = np.where(rel >= 0, 0.0, -1e30)
    return t5.astype(bf16), sbm.astype(bf16), cm.astype(bf16)


_NC_CACHE = {}
_DEBUG = False


def kernel(x, w_in, w_branch_a, w_branch_b, w_out, rel_bias, ln1_g, ln1_b, w_router, b_router,
           w_gate_up, b_gate_up, w_down, b_down, ln2_g, ln2_b):
    x = np.asarray(x, np.float32)
    w = np.asarray(w_in, np.float32)[0]
    cs = np.cumsum([0, 512, 512, 512, 512, 64, 8, 512, 512, 512, 1024, 1024])
    qa, ka, va, qi, ki, wi, qb, kb, vb, ga, gb = [w[:, cs[i]:cs[i + 1]] for i in range(11)]
    shared = dict(
        wKA=np.ascontiguousarray(np.concatenate([ka, ki, ki, va], 1)),
        wKB=np.ascontiguousarray(np.concatenate([kb, vb], 1)),
        wQA=np.ascontiguousarray(np.concatenate([qa, qi], 1)),
        wWI=np.ascontiguousarray(wi),
        wQB=np.ascontiguousarray(qb),
        wG=np.ascontiguousarray(np.concatenate([ga, gb], 1)),
        wA=np.asarray(w_branch_a, np.float32)[0], wB=np.asarray(w_branch_b, np.float32)[0],
        wO=np.asarray(w_out, np.float32)[0], wR=np.asarray(w_router, np.float32)[0],
        wGU=np.asarray(w_gate_up, np.float32)[0], wDN=np.asarray(w_down, np.float32)[0],
        bGU=np.ascontiguousarray(np.asarray(b_gate_up, np.float32)[0].reshape(NE, 16, 128).transpose(2, 0, 1).reshape(128, NE * 16)),
        bDN=np.asarray(b_down, np.float32)[0],
        bRT=np.ascontiguousarray(np.broadcast_to(np.asarray(b_router, np.float32)[0][None, :], (128, NE))),
        lnp=np.ascontiguousarray(np.stack([np.broadcast_to(np.asarray(a, np.float32)[0][None, :], (128, D))
                                           for a in (ln1_g, ln1_b, ln2_g, ln2_b)])),
        rb31=np.ascontiguousarray(np.broadcast_to(np.asarray(rel_bias, np.float32)[31][None, :], (128, 8))),
    )
    eye = np.eye(128, dtype=np.float32)
    jj = np.arange(128)[:, None]
    ss = np.arange(128)[None, :]
    negU = np.where(jj >= ss, -1.0, 0.0)
    ustrict = np.where(jj < ss, 1.0, 0.0)
    shared["cst_b"] = np.concatenate([eye, negU, -np.ones((128, 128)), ustrict, np.ones((128, 128))], 1).astype(bf16)
    pow2 = np.broadcast_to((2.0 ** -np.arange(NIT + 1))[None, :], (128, NIT + 1))
    iota = np.broadcast_to(np.arange(CAP)[None, :], (128, CAP))
    shared["cst_f"] = np.ascontiguousarray(np.concatenate([eye, pow2, iota], 1).astype(np.float32))
    rb = np.asarray(rel_bias, np.float32)
    tabs = {hf: _host_tables(rb, hf) for hf in (0, 1)}
    in_maps = []
    for c in range(NCORES):
        b, hf = c // 2, c % 2
        cols = np.concatenate([np.arange(g * TQ, (g + 1) * TQ) for g in GROUPS[hf]])
        xb = x[b]
        m = dict(shared)
        m["xT"] = np.ascontiguousarray(xb.T)
        m["xTq"] = np.ascontiguousarray(xb[cols].T)
        m["xq"] = np.ascontiguousarray(xb[cols])
        m["t5tab"], m["sbmask"], m["cmask"] = tabs[hf]
        in_maps.append(m)
    if "nc" not in _NC_CACHE:
        _NC_CACHE["nc"] = build_program(debug=_DEBUG)
    res = run_bass_kernel_spmd(_NC_CACHE["nc"], in_maps, core_ids=list(range(NCORES)))
    if _DEBUG:
        _NC_CACHE["res"] = res
    outp = np.empty((4, S, D), np.float32)
    for c in range(NCORES):
        b, hf = c // 2, c % 2
        cols = np.concatenate([np.arange(g * TQ, (g + 1) * TQ) for g in GROUPS[hf]])
        outp[b, cols] = res.results[c]["out"]
    return outp
```

```python
import math
from contextlib import ExitStack

import numpy as np
import ml_dtypes

import concourse.bass as bass
import concourse.mybir as mybir
from concourse.bass_utils import run_bass_kernel_spmd

F32 = mybir.dt.float32
BF16 = mybir.dt.bfloat16
U8 = mybir.dt.uint8
ALU = mybir.AluOpType
AF = mybir.ActivationFunctionType
bf16 = ml_dtypes.bfloat16

NCORES = 8
S = 4096
D = 1024
NLG = 4
TQ = 512
NOWN = NLG * TQ
NBLK = NOWN // 128
NE = 32
CAP = 320
CBS = [(0, 128), (128, 128), (256, 64)]
NIT = 14
NEG = -30000.0
ALPHA = 2.0 ** 0.25
LN_EPS = 1e-5
GROUPS = {0: [0, 3, 4, 7], 1: [1, 2, 5, 6]}

ENGS = ("pe", "act", "dve", "pool", "sp")
NDMASEM = 12


class Prog:
    def __init__(self, nc, stack):
        self.nc = nc
        self.ops = []
        self.sems = {e: stack.enter_context(nc.semaphore("s_" + e)) for e in ENGS}
        self.dsems = {q: [stack.enter_context(nc.semaphore("d_%s_%d" % (q, r))) for r in range(NDMASEM)]
                      for q in ("sp", "act", "pool")}
        self.cnt = {e: 0 for e in ENGS}
        self.dcnt = {q: 0 for q in self.dsems}
        self.waited_d = {e: {} for e in ENGS}
        self.nops = 0

    def op(self, eng, fn, reads=(), writes=(), dma=False, big=False):
        self.ops.append(dict(eng=eng, fn=fn, reads=tuple(reads), writes=tuple(writes), dma=dma, big=big))

    def dma(self, q, out, in_, reads=(), writes=()):
        self.op(q, lambda e: e.dma_start(out=out, in_=in_), reads, writes, dma=True)

    def emit(self):
        nc = self.nc
        dkeys = set()
        for o in self.ops:
            if o["dma"]:
                dkeys.update(o["writes"])
        self.op("sp", None, reads=sorted(dkeys, key=str))
        ops = self.ops
        n = len(ops)
        self.nops += n
        last_w, readers = {}, {}
        deps = [None] * n
        raw = [None] * n
        for i, o in enumerate(ops):
            d = set()
            for k in o["reads"]:
                if k in last_w:
                    d.add(last_w[k])
            raw[i] = set(d)
            for k in o["writes"]:
                if k in last_w:
                    d.add(last_w[k])
                for r in readers.get(k, ()):
                    d.add(r)
            d.discard(i)
            deps[i] = d
            for k in o["writes"]:
                last_w[k] = i
                readers[k] = []
            for k in o["reads"]:
                readers.setdefault(k, []).append(i)
        seen = {e: {f: -1 for f in ENGS} for e in ENGS}
        need = [None] * n
        signal = [False] * n
        for i, o in enumerate(ops):
            E = o["eng"]
            best, keep = {}, []
            for j in deps[i]:
                oj = ops[j]
                if oj["dma"]:
                    keep.append(j)
                    continue
                Fe = oj["eng"]
                if Fe == E:
                    if E in ("pe", "sp"):
                        continue
                if seen[E][Fe] >= j:
                    continue
                if Fe not in best or best[Fe] < j:
                    best[Fe] = j
            for Fe, j in best.items():
                keep.append(j)
                seen[E][Fe] = j
                signal[j] = True
            need[i] = sorted(keep)
        need[n - 1] = sorted(set(need[n - 1]) | {i for i in range(n) if ops[i]["dma"]})
        sigval = [0] * n
        dinfo = {}
        acts = {e: [] for e in ENGS}
        for i, o in enumerate(ops):
            E = o["eng"]
            for j in need[i]:
                oj = ops[j]
                if oj["dma"]:
                    s, v, key = dinfo[j]
                    if self.waited_d[E].get(key, 0) >= v:
                        continue
                    self.waited_d[E][key] = v
                    acts[E].append(("w", s, v))
                else:
                    acts[E].append(("w", self.sems[oj["eng"]], sigval[j]))
            if o["dma"]:
                k = self.dcnt[E]
                self.dcnt[E] += 1
                r = k % NDMASEM
                s = self.dsems[E][r]
                if k >= NDMASEM:
                    v0 = 16 * (k // NDMASEM)
                    key = (E, r)
                    if self.waited_d[E].get(key, 0) < v0:
                        acts[E].append(("w", s, v0))
                        self.waited_d[E][key] = v0
                acts[E].append(("o", o["fn"], s, 16))
                dinfo[i] = (s, 16 * (k // NDMASEM + 1), (E, r))
            elif o["fn"] is not None:
                if signal[i]:
                    self.cnt[E] += 1
                    sigval[i] = self.cnt[E]
                    acts[E].append(("o", o["fn"], self.sems[E], 1))
                else:
                    acts[E].append(("o", o["fn"], None, 0))

        def replay(E):
            def f(e):
                for a in acts[E]:
                    if a[0] == "w":
                        e.wait_ge(a[1], a[2])
                    else:
                        inst = a[1](e)
                        if a[2] is not None:
                            inst.then_inc(a[2], a[3])
            return f

        with nc.Block() as block:
            block.tensor(replay("pe"))
            block.scalar(replay("act"))
            block.vector(replay("dve"))
            block.gpsimd(replay("pool"))
            block.sync(replay("sp"))
        nc.all_engine_barrier()
        self.ops = []


class Ring:
    def __init__(self, tiles, name):
        self.tiles = tiles
        self.name = name
        self.i = 0

    def next(self):
        k = self.i % len(self.tiles)
        self.i += 1
        return self.tiles[k], "%s%d" % (self.name, k)


def mm_group(P, out, pairs, reads, wkey, start=True, stop=True):
    def fn(e):
        inst = None
        n = len(pairs)
        for i, p in enumerate(pairs):
            if len(p) == 3:
                o, l, r = p
            else:
                o = out
                l, r = p
            inst = e.matmul(o, lhsT=l, rhs=r, start=(start and i == 0), stop=(stop and i == n - 1),
                            skip_group_check=True)
        return inst
    P.op("pe", fn, reads=reads, writes=[wkey])


def build_program(debug=False):
    nc = bass.Bass("TRN2", target_bir_lowering=False)

    def din(name, shape, dt=F32):
        return nc.dram_tensor(name, list(shape), dt, kind="ExternalInput").ap()

    xT = din("xT", [D, S])
    xTq = din("xTq", [D, NOWN])
    xq = din("xq", [NOWN, D])
    wKA = din("wKA", [D, 1152])
    wKB = din("wKB", [D, 1024])
    wQA = din("wQA", [D, 1024])
    wWI = din("wWI", [D, 8])
    wQB = din("wQB", [D, 512])
    wG = din("wG", [D, 2048])
    wA = din("wA", [512, D])
    wB = din("wB", [512, D])
    wO = din("wO", [D, D])
    wR = din("wR", [D, NE])
    wGU = din("wGU", [NE, D, 2048])
    wDN = din("wDN", [NE, D, D])
    bGU = din("bGU", [128, NE * 16])
    bDN = din("bDN", [NE, D])
    bRT = din("bRT", [128, NE])
    lnp = din("lnp", [4, 128, D])
    rb31 = din("rb31", [128, 8])
    t5tab = din("t5tab", [2, 8, 9, 128, TQ], BF16)
    sbmask = din("sbmask", [2, 8, 128, TQ], BF16)
    cmask = din("cmask", [2, 4, 128, 1024], BF16)
    cst_b = din("cst_b", [128, 5 * 128], BF16)
    cst_f = din("cst_f", [128, 128 + NIT + 1 + CAP])
    out = nc.dram_tensor("out", [NOWN, D], F32, kind="ExternalOutput").ap()
    dk = dict(kind="ExternalOutput") if debug else {}
    ya_d = nc.dram_tensor("ya_d", [512, NOWN], BF16, **dk).ap()
    yb_d = nc.dram_tensor("yb_d", [512, NOWN], BF16, **dk).ap()
    h1_d = nc.dram_tensor("h1_d", [NOWN, D], F32, **dk).ap()
    if debug:
        dbg_f = nc.dram_tensor("dbg_f", [128, NBLK, D], F32, kind="ExternalOutput").ap()
        dbg_g = nc.dram_tensor("dbg_g", [128, NBLK, NE], F32, kind="ExternalOutput").ap()
        dbg_p = nc.dram_tensor("dbg_p", [128, NBLK, NE], F32, kind="ExternalOutput").ap()
        dbg_x = nc.dram_tensor("dbg_x", [128, 8, CAP], BF16, kind="ExternalOutput").ap()
        dbg_a = nc.dram_tensor("dbg_a", [128, 8, CAP], BF16, kind="ExternalOutput").ap()
        dbg_y = nc.dram_tensor("dbg_y", [128, 3, D], BF16, kind="ExternalOutput").ap()

    top = ExitStack()
    with top:
        P = Prog(nc, top)

        def sb(st, name, shape, dt):
            return st.enter_context(nc.sbuf_tensor(name, list(shape), dt))

        def ps(st, name, shape=(128, 512), dt=F32):
            return st.enter_context(nc.psum_tensor(name, list(shape), dt))

        cb = sb(top, "cb", [128, 5 * 128], BF16)
        cf = sb(top, "cf", [128, 128 + NIT + 1 + CAP], F32)
        ident, negU, negones, ustrict, ones = [cb[:, i * 128:(i + 1) * 128] for i in range(5)]
        identf = cf[:, 0:128]
        pow2 = cf[:, 128:128 + NIT + 1]
        iota = cf[:, 128 + NIT + 1:128 + NIT + 1 + CAP]
        gates_all = sb(top, "gates_all", [128, NBLK, NE], F32)
        mask_b = sb(top, "mask_b", [128, NBLK, NE], BF16)
        mask_f = sb(top, "mask_f", [128, NBLK, NE], F32)
        rb31s = sb(top, "rb31s", [128, 8], F32)
        P.dma("sp", cb[:], cst_b[:, :], writes=["cb"])
        P.dma("sp", cf[:], cst_f[:, :], writes=["cf"])
        P.dma("sp", rb31s[:], rb31[:, :], writes=["rb31s"])
        psb = [ps(top, "psb%d" % i) for i in range(8)]

        def kside(st, wsrc, ncols_fm, fm_dst, tm_dst, tag):
            ncol = ncols_fm + 512
            w = sb(st, "wk" + tag, [128, 8, ncol], BF16)
            P.dma("pool", w[:], wsrc.rearrange("(c p) n -> p c n", p=128), writes=["wk"])
            xr = Ring([sb(st, "xk%s%d" % (tag, i), [128, 8, 512], BF16) for i in range(2)], "xk")
            pr = Ring(psb[0:4], "psb")
            pr.i = 0
            xTv = xT.rearrange("(c p) t -> p c t", p=128)
            for tc in range(8):
                xt, xk = xr.next()
                P.dma("pool", xt[:], xTv[:, :, tc * 512:(tc + 1) * 512], writes=[xk])
                for oc in range(ncols_fm // 128):
                    pt, pk = next_ps(pr)
                    mm_group(P, pt[:], [(w[:, dc, oc * 128:(oc + 1) * 128], xt[:, dc, :]) for dc in range(8)],
                             ["wk", xk], pk)
                    dst, dk = fm_dst(oc, tc)
                    P.op("act", lambda e, d=dst, p=pt: e.activation(out=d, in_=p[:], func=AF.Copy),
                         reads=[pk], writes=[dk])
                for tb in range(4):
                    pt, pk = next_ps(pr)
                    mm_group(P, pt[:], [(xt[:, dc, tb * 128:(tb + 1) * 128], w[:, dc, ncols_fm:ncol]) for dc in range(8)],
                             ["wk", xk], pk)
                    dst, dk = tm_dst(tc * 4 + tb)
                    P.op("dve", lambda e, d=dst, p=pt: e.tensor_copy(out=d, in_=p[:]), reads=[pk], writes=[dk])

        def next_ps(pr):
            t, k = pr.next()
            return t, k

        xTqv = xTq.rearrange("(c p) t -> p c t", p=128)

        with ExitStack() as st:
            kaT = sb(st, "kaT", [128, 4, S], BF16)
            kiT = sb(st, "kiT", [128, S], BF16)
            va = sb(st, "va", [128, 32, 512], BF16)
            with ExitStack() as st0:
                kside(st0, wKA, 640,
                      lambda oc, tc: ((kaT[:, oc, tc * 512:(tc + 1) * 512], "kaT") if oc < 4
                                      else (kiT[:, tc * 512:(tc + 1) * 512], "kiT")),
                      lambda blk: (va[:, blk, :], "va"), "A")
                P.emit()
            wq = sb(st, "wqa", [128, 8, 1024], BF16)
            wwi = sb(st, "wwi", [128, 8, 8], BF16)
            P.dma("pool", wq[:], wQA.rearrange("(c p) n -> p c n", p=128), writes=["wq"])
            P.dma("pool", wwi[:], wWI.rearrange("(c p) n -> p c n", p=128), writes=["wwi"])
            xqb = sb(st, "xqb", [128, 8, TQ], BF16)
            qaT = sb(st, "qaT", [128, 4, TQ], BF16)
            qiT = sb(st, "qiT", [128, 4, TQ], BF16)
            wis = sb(st, "wis", [128, 4, 8], F32)
            diagr = Ring([sb(st, "diag%d" % i, [128, 8, 128], BF16) for i in range(2)], "diag")
            Rr = Ring([sb(st, "R%d" % i, [128, 512], BF16) for i in range(4)], "R")
            Isbr = Ring([sb(st, "Isb%d" % i, [128, S], F32) for i in range(2)], "Isb")
            junk = sb(st, "junk", [128, S], U8)
            mbias = [sb(st, "mbias%d" % i, [128, S], BF16) for i in range(4)]
            cmr = Ring([sb(st, "cm%d" % i, [128, 1024], BF16) for i in range(2)], "cm")
            bis = sb(st, "bis", [128, 8 + NIT + 1], F32)
            bisa = sb(st, "bisa", [128, 8 + NIT + 1], F32)
            t5r = Ring([sb(st, "t5_%d" % i, [128, TQ], BF16) for i in range(4)], "t5")
            pr_ = Ring([sb(st, "p%d" % i, [128, TQ], BF16) for i in range(3)], "p")
            rden = sb(st, "rden", [64, TQ], F32)
            yor = Ring([sb(st, "yo%d" % i, [64, TQ], BF16) for i in range(2)], "yo")
            zar = Ring(psb[0:3], "psb")
            Yr = Ring(psb[4:6], "psY")
            Dr = Ring(psb[6:8], "psD")

            class _IR:
                i = 0

                def next(self):
                    self.i += 1
                    return (psb[3], "psb3") if self.i % 2 else (psb[7], "psD1")
            Ipr = _IR()
            for lg in range(NLG):
                par = lg % 2
                nkb = 8 * (lg + 1)
                Slg = 128 * nkb
                P.dma("pool", xqb[:], xTqv[:, :, lg * TQ:(lg + 1) * TQ], writes=["xqb"])
                for oc in range(8):
                    pt, pk = zar.next()
                    mm_group(P, pt[:], [(wq[:, dc, oc * 128:(oc + 1) * 128], xqb[:, dc, :]) for dc in range(8)],
                             ["wq", "xqb"], pk)
                    if oc < 4:
                        P.op("act", lambda e, d=qaT[:, oc, :], p=pt: e.mul(d, p[:], 0.125),
                             reads=[pk], writes=["qaT"])
                    else:
                        P.op("act", lambda e, d=qiT[:, oc - 4, :], p=pt: e.activation(out=d, in_=p[:], func=AF.Copy),
                             reads=[pk], writes=["qiT"])
                pt, pk = zar.next()
                for tb in range(4):
                    mm_group(P, pt[:, tb * 8:(tb + 1) * 8],
                             [(xqb[:, dc, tb * 128:(tb + 1) * 128], wwi[:, dc, :]) for dc in range(8)],
                             ["wwi", "xqb"], pk)
                P.op("dve", lambda e, p=pt: e.tensor_copy(out=wis[:].rearrange("p a b -> p (a b)"), in_=p[:, 0:32]),
                     reads=[pk], writes=["wis"])
                Istate = {}

                def IDX(tb, lg=lg, par=par, nkb=nkb):
                    dg, dgk = diagr.next()
                    for h in range(8):
                        P.op("pool", lambda e, d=dg[:, h, :], s=wis[:, tb, h:h + 1]: e.tensor_scalar(
                            out=d, in0=ident, scalar1=s, scalar2=None, op0=ALU.mult),
                            reads=["wis", "cb"], writes=[dgk])
                    cm, cmk = cmr.next()
                    P.dma("sp", cm[:], cmask[par, tb], writes=[cmk])
                    Isb, Isbk = Isbr.next()
                    nsc = nkb // 4
                    U = nsc * 8
                    units = [dict(sc=u // 8, h=u % 8) for u in range(U)]
                    Ist = {}

                    def Zf(u):
                        Uu = units[u]
                        h, sc = Uu["h"], Uu["sc"]
                        hp, hc = (h % 2) * 64, h // 2
                        Uu["zt"], Uu["zk"] = zar.next()
                        mm_group(P, Uu["zt"][:], [(qiT[hp:hp + 64, hc, tb * 128:(tb + 1) * 128],
                                                   kiT[hp:hp + 64, sc * 512:(sc + 1) * 512])], ["qiT", "kiT"], Uu["zk"])

                    def Rf(u):
                        Uu = units[u]
                        Uu["rt"], Uu["rk"] = Rr.next()
                        P.op("act", lambda e, d=Uu["rt"], p=Uu["zt"]: e.activation(out=d[:], in_=p[:], func=AF.Relu),
                             reads=[Uu["zk"]], writes=[Uu["rk"]])

                    def Df(u):
                        Uu = units[u]
                        h, sc = Uu["h"], Uu["sc"]
                        if h == 0:
                            Ist[sc] = Ipr.next()
                        Ips, Ik = Ist[sc]
                        mm_group(P, Ips[:], [(dg[:, h, :], Uu["rt"][:])], [dgk, Uu["rk"]], Ik, start=(h == 0), stop=(h == 7))
                        if h == 7:
                            dst = Isb[:, sc * 512:(sc + 1) * 512]
                            if sc >= 2 * lg:
                                c0 = (sc - 2 * lg) * 512
                                P.op("dve", lambda e, d=dst, c=cm[:, c0:c0 + 512], Ips=Ips: e.tensor_tensor(out=d, in0=Ips[:], in1=c, op=ALU.add),
                                     reads=[Ik, cmk], writes=[Isbk])
                            else:
                                P.op("dve", lambda e, d=dst, Ips=Ips: e.tensor_copy(out=d, in_=Ips[:]), reads=[Ik], writes=[Isbk])

                    for u in range(-2, U):
                        if 0 <= u + 2 < U:
                            Zf(u + 2)
                        if 0 <= u + 1 < U:
                            Rf(u + 1)
                        if 0 <= u < U:
                            Df(u)
                    Istate[tb] = (Isb, Isbk)

                def BIS(tb, which, Slg=Slg):
                    Isb, Isbk = Istate[tb]
                    Iv = Isb[:, 0:Slg]
                    mbk = "mbias%d" % tb
                    if which == 0:
                        bs, pfx, jv, jk = bis, "b", junk[:, 0:Slg], "junk"
                    else:
                        bs, pfx, jv, jk = bisa, "a", mbias[tb][:, 0:Slg], mbk
                    K = lambda n: pfx + n
                    L = []
                    A = lambda fn, reads, writes: L.append(lambda: P.op("dve", fn, reads=reads, writes=writes))
                    A(lambda e: e.reduce_max(out=bs[:, 0:1], in_=Iv, axis=mybir.AxisListType.X), [Isbk], [K("B")])
                    A(lambda e: e.tensor_scalar(out=bs[:, 5:6], in0=bs[:, 0:1], scalar1=-1.0, scalar2=None, op0=ALU.mult), [K("B")], [K("N")])
                    A(lambda e: e.tensor_tensor(out=bs[:, 6:7], in0=bs[:, 0:1], in1=bs[:, 5:6], op=ALU.max), [K("B"), K("N")], [K("A")])
                    A(lambda e: e.tensor_scalar(out=bs[:, 7:8], in0=bs[:, 6:7], scalar1=1.0, scalar2=2.0, op0=ALU.max, op1=ALU.mult), [K("A")], [K("R")])
                    A(lambda e: e.tensor_scalar(out=bs[:, 8:8 + NIT + 1], in0=pow2, scalar1=bs[:, 7:8], scalar2=None, op0=ALU.mult), [K("R"), "cf"], [K("steps")])
                    A(lambda e: e.memset(bs[:, 1:2], 0.0), [], [K("cand")])
                    for k in range(NIT):
                        A(lambda e: e.tensor_scalar(out=jv, in0=Iv, scalar1=bs[:, 1:2], scalar2=None, op0=ALU.is_ge, op1=ALU.add, accum_out=bs[:, 2:3]),
                          [Isbk, K("cand")], [K("cnt"), jk])
                        A(lambda e, k=k: e.scalar_tensor_tensor(out=bs[:, 3:4], in0=bs[:, 2:3], scalar=256.0, in1=bs[:, 8 + k:9 + k], op0=ALU.is_ge, op1=ALU.mult),
                          [K("cnt"), K("steps")], [K("inc")])
                        A(lambda e, k=k: e.scalar_tensor_tensor(out=bs[:, 1:2], in0=bs[:, 3:4], scalar=bs[:, 9 + k:10 + k], in1=bs[:, 1:2], op0=ALU.subtract, op1=ALU.add),
                          [K("inc"), K("steps"), K("cand")], [K("cand")])
                    A(lambda e: e.tensor_tensor(out=bs[:, 4:5], in0=bs[:, 1:2], in1=bs[:, 8 + NIT:9 + NIT], op=ALU.subtract), [K("cand"), K("steps")], [K("thr")])
                    A(lambda e: e.tensor_scalar(out=bs[:, 5:6], in0=bs[:, 7:8], scalar1=1.0 - 2.0 ** -(NIT + 1), scalar2=None, op0=ALU.mult), [K("R"), K("A")], [K("M")])
                    A(lambda e: e.tensor_tensor(out=bs[:, 6:7], in0=bs[:, 4:5], in1=bs[:, 5:6], op=ALU.add), [K("thr"), K("M"), K("A")], [K("T1")])
                    A(lambda e: e.tensor_scalar(out=bs[:, 6:7], in0=bs[:, 6:7], scalar1=0.0, scalar2=-1e29, op0=ALU.is_le, op1=ALU.mult), [K("T1")], [K("Pen")])
                    A(lambda e: e.tensor_tensor(out=bs[:, 3:4], in0=bs[:, 4:5], in1=bs[:, 6:7], op=ALU.add), [K("thr"), K("Pen"), K("inc")], [K("thr2")])
                    A(lambda e, d=mbias[tb][:, 0:Slg]: e.tensor_scalar(out=d, in0=Iv, scalar1=bs[:, 3:4], scalar2=NEG, op0=ALU.is_lt, op1=ALU.mult),
                      [Isbk, K("thr2")], [mbk])
                    return L

                for t0 in (0, 2):
                    IDX(t0)
                    IDX(t0 + 1)
                    La, Lb = BIS(t0, 0), BIS(t0 + 1, 1)
                    for fa, fb in zip(La, Lb):
                        fa()
                        fb()
                blocks = [dict(h=h, kb=kb) for h in range(8) for kb in range(nkb)]
                NB = len(blocks)
                YD = {}

                def Af(b):
                    B = blocks[b]
                    h, kb = B["h"], B["kb"]
                    hp, hc = (h % 2) * 64, h // 2
                    near = kb >= 8 * lg - 1
                    B["near"] = near
                    B["at"], B["ak"] = zar.next()
                    at = B["at"]
                    pairs = [(at[:], kaT[hp:hp + 64, hc, kb * 128:(kb + 1) * 128], qaT[hp:hp + 64, hc, :])]
                    rd = ["kaT", "qaT", "cb"] + ["mbias%d" % t for t in range(4)]
                    for tb in range(4):
                        pairs.append((at[:, tb * 128:(tb + 1) * 128], mbias[tb][:, kb * 128:(kb + 1) * 128], ident))
                    if near:
                        t5, t5k = t5r.next()
                        P.dma("sp", t5[:], t5tab[par, h, kb - 8 * lg + 1], writes=[t5k])
                        pairs.append((at[:], ident, t5[:]))
                        rd.append(t5k)
                    mm_group(P, at[:], pairs, rd, B["ak"])

                def Xf(b):
                    B = blocks[b]
                    h = B["h"]
                    B["pt"], B["pk"] = pr_.next()
                    if B["near"]:
                        P.op("act", lambda e, d=B["pt"], a=B["at"]: e.activation(out=d[:], in_=a[:], func=AF.Exp),
                             reads=[B["ak"]], writes=[B["pk"]])
                    else:
                        P.op("act", lambda e, d=B["pt"], a=B["at"], b_=rb31s[:, h:h + 1]: e.activation(out=d[:], in_=a[:], func=AF.Exp, bias=b_),
                             reads=[B["ak"], "rb31s"], writes=[B["pk"]])

                def Vf(b):
                    B = blocks[b]
                    h, kb = B["h"], B["kb"]
                    if kb == 0:
                        YD[h] = (Yr.next(), Dr.next())
                    (Yt, Yk), (Dt, Dk) = YD[h]
                    mm_group(P, Yt[0:64, :], [(va[:, kb, h * 64:(h + 1) * 64], B["pt"][:])], ["va", B["pk"]], Yk,
                             start=(kb == 0), stop=(kb == nkb - 1))
                    mm_group(P, Dt[0:64, :], [(ones[:, 0:64], B["pt"][:])], ["cb", B["pk"]], Dk,
                             start=(kb == 0), stop=(kb == nkb - 1))
                    if kb == nkb - 1:
                        P.op("dve", lambda e, d=Dt: e.reciprocal(out=rden[:], in_=d[0:64, :]), reads=[Dk], writes=["rden"])
                        yo, yok = yor.next()
                        P.op("dve", lambda e, y=Yt, o=yo: e.tensor_tensor(out=o[:], in0=y[0:64, :], in1=rden[:], op=ALU.mult),
                             reads=[Yk, "rden"], writes=[yok])
                        P.dma("sp", ya_d[h * 64:(h + 1) * 64, lg * TQ:(lg + 1) * TQ], yo[:], reads=[yok], writes=["ya_d%d_%d" % (lg, h)])

                for i in range(-2, NB):
                    if 0 <= i + 2 < NB:
                        Af(i + 2)
                    if 0 <= i + 1 < NB:
                        Xf(i + 1)
                    if 0 <= i < NB:
                        Vf(i)
            P.emit()

        with ExitStack() as st:
            kbT = sb(st, "kbT", [128, 4, S], BF16)
            vb = sb(st, "vb", [128, 32, 512], BF16)
            with ExitStack() as st0:
                kside(st0, wKB, 512, lambda oc, tc: (kbT[:, oc, tc * 512:(tc + 1) * 512], "kbT"),
                      lambda blk: (vb[:, blk, :], "vb"), "B")
                P.emit()
            wq = sb(st, "wqb", [128, 8, 512], BF16)
            P.dma("pool", wq[:], wQB.rearrange("(c p) n -> p c n", p=128), writes=["wq"])
            xqb = sb(st, "xqb2", [128, 8, TQ], BF16)
            qbT = sb(st, "qbT", [128, 4, TQ], BF16)
            sbm = sb(st, "sbm", [128, 8, TQ], BF16)
            er = Ring([sb(st, "e%d" % i, [128, TQ], F32) for i in range(3)], "e")
            spr = Ring([sb(st, "sp%d" % i, [128, TQ], BF16) for i in range(3)], "spl")
            wr_ = Ring([sb(st, "w%d" % i, [128, TQ], BF16) for i in range(3)], "w")
            accr = Ring([sb(st, "acc%d" % i, [128, TQ], BF16) for i in range(2)], "acc")
            yor = Ring([sb(st, "yob%d" % i, [64, TQ], BF16) for i in range(2)], "yob")
            ar = Ring(psb[0:4] + [psb[6], psb[7]], "psA")
            Yr = Ring(psb[4:6], "psY")
            for lg in range(NLG):
                par = lg % 2
                nkb = 8 * (lg + 1)
                P.dma("pool", xqb[:], xTqv[:, :, lg * TQ:(lg + 1) * TQ], writes=["xqb"])
                P.dma("sp", sbm[:], sbmask[par].rearrange("j p t -> p j t"), writes=["sbm"])
                for oc in range(4):
                    pt, pk = ar.next()
                    mm_group(P, pt[:], [(wq[:, dc, oc * 128:(oc + 1) * 128], xqb[:, dc, :]) for dc in range(8)],
                             ["wq", "xqb"], pk)
                    P.op("act", lambda e, d=qbT[:, oc, :], p=pt: e.mul(d, p[:], 0.125),
                         reads=[pk], writes=["qbT"])
                blocks = []
                for h in range(8):
                    for idx, kb in enumerate(range(nkb - 1, -1, -1)):
                        blocks.append(dict(h=h, idx=idx, kb=kb, first=(idx == 0), last=(idx == nkb - 1)))
                NB = len(blocks)
                Ystate = {}

                def S1(b):
                    B = blocks[b]
                    h, kb = B["h"], B["kb"]
                    hp, hc = (h % 2) * 64, h // 2
                    B["at"], B["ak"] = ar.next()
                    pairs = [(kbT[hp:hp + 64, hc, kb * 128:(kb + 1) * 128], qbT[hp:hp + 64, hc, :])]
                    rd = ["kbT", "qbT"]
                    if kb >= 8 * lg:
                        pairs.append((ident, sbm[:, kb - 8 * lg, :]))
                        rd += ["cb", "sbm"]
                    mm_group(P, B["at"][:], pairs, rd, B["ak"], stop=False)

                def E1(b):
                    B = blocks[b]
                    B["et"], B["ek"] = er.next()
                    P.op("act", lambda e, d=B["et"], a=B["at"]: e.activation(out=d[:], in_=a[:], func=AF.Exp), reads=[B["ak"]], writes=[B["ek"]], big=True)

                def Lacc(b):
                    B = blocks[b]
                    et, ek = B["et"], B["ek"]
                    B["sp"], B["spk"] = spr.next()
                    P.op("act", lambda e, d=B["sp"], a=et: e.activation(out=d[:], in_=a[:], func=AF.Ln, bias=1.0, scale=1.0),
                         reads=[ek], writes=[B["spk"]], big=True)
                    if not B["last"]:
                        acn, acnk = accr.next()
                        if B["first"]:
                            P.op("pool", lambda e, d=acn, s_=B["sp"]: e.tensor_copy(out=d[:], in_=s_[:]), reads=[B["spk"]], writes=[acnk])
                        else:
                            pa_, pak_ = B["acc"]
                            P.op("pool", lambda e, d=acn, s_=B["sp"], a=pa_: e.tensor_tensor(out=d[:], in0=a[:], in1=s_[:], op=ALU.add),
                                 reads=[B["spk"], pak_], writes=[acnk])
                        blocks[b + 1]["acc"] = (acn, acnk)

                def S2(b):
                    B = blocks[b]
                    pairs = [(negU, B["sp"][:])]
                    rd = ["cb", B["spk"]]
                    if not B["first"]:
                        pairs.append((negones, B["acc"][0][:]))
                        rd.append(B["acc"][1])
                    mm_group(P, B["at"][:], pairs, rd, B["ak"], start=False)

                def E2(b):
                    B = blocks[b]
                    B["w"], B["wk"] = wr_.next()
                    P.op("act", lambda e, d=B["w"], a=B["at"]: e.activation(out=d[:], in_=a[:], func=AF.Exp), reads=[B["ak"]], writes=[B["wk"]], big=True)

                def S3(b):
                    B = blocks[b]
                    h, kb = B["h"], B["kb"]
                    if B["first"]:
                        Ystate[h] = Yr.next()
                    Yt, Yk = Ystate[h]
                    mm_group(P, Yt[0:64, :], [(vb[:, kb, h * 64:(h + 1) * 64], B["w"][:])], ["vb", B["wk"]], Yk,
                             start=B["first"], stop=B["last"])
                    if B["last"]:
                        yo, yok = yor.next()
                        P.op("dve", lambda e, y=Yt, o=yo: e.tensor_copy(out=o[:], in_=y[0:64, :]), reads=[Yk], writes=[yok])
                        P.dma("sp", yb_d[h * 64:(h + 1) * 64, lg * TQ:(lg + 1) * TQ], yo[:], reads=[yok], writes=["yb_d%d_%d" % (lg, h)])

                for i in range(-3, NB + 1):
                    if 0 <= i < NB:
                        S2(i)
                    if 0 <= i + 3 < NB:
                        S1(i + 3)
                    if 0 <= i - 1 < NB:
                        S3(i - 1)
                    if 0 <= i + 2 < NB:
                        E1(i + 2)
                    if 0 <= i + 1 < NB:
                        Lacc(i + 1)
                    if 0 <= i < NB:
                        E2(i)
            P.emit()

        with ExitStack() as st:
            wg = sb(st, "wg", [128, 8, 2048], BF16)
            wa = sb(st, "wa", [128, 4, D], BF16)
            wb = sb(st, "wb", [128, 4, D], BF16)
            wo = sb(st, "wo", [128, 8, D], BF16)
            wr = sb(st, "wr", [128, 8, NE], F32)
            brt = sb(st, "brt", [128, NE], F32)
            lnps = sb(st, "lnps", [128, 2, D], F32)
            P.dma("pool", wg[:], wG.rearrange("(c p) n -> p c n", p=128), writes=["wg"])
            P.dma("pool", wa[:], wA.rearrange("(c p) n -> p c n", p=128), writes=["wa"])
            P.dma("pool", wb[:], wB.rearrange("(c p) n -> p c n", p=128), writes=["wb"])
            P.dma("pool", wo[:], wO.rearrange("(c p) n -> p c n", p=128), writes=["wo"])
            P.dma("sp", wr[:], wR.rearrange("(c p) n -> p c n", p=128), writes=["wr"])
            P.dma("sp", brt[:], bRT[:, :], writes=["brt"])
            P.dma("sp", lnps[:], lnp[0:2].rearrange("a p d -> p a d"), writes=["lnps"])
            xqb = sb(st, "xqb3", [128, 8, TQ], BF16)
            yas = sb(st, "yas", [128, 4, TQ], BF16)
            ybs = sb(st, "ybs", [128, 4, TQ], BF16)
            sg = sb(st, "sg", [128, 16, TQ], BF16)
            mT = sb(st, "mT", [128, 8, TQ], BF16)
            t1 = sb(st, "t1", [128, TQ], F32)
            t2 = sb(st, "t2", [128, TQ], F32)
            xres = sb(st, "xres", [128, D], F32)
            r1 = sb(st, "r1", [128, D], F32)
            sq = sb(st, "sq", [128, D], F32)
            h1t = sb(st, "h1t", [128, D], F32)
            h1T = sb(st, "h1T", [128, 8, 128], F32)
            sm = sb(st, "sm", [128, 64], F32)
            pr = Ring(psb, "psb")
            for lg in range(NLG):
                P.dma("pool", xqb[:], xTqv[:, :, lg * TQ:(lg + 1) * TQ], writes=["xqb"])
                P.dma("sp", yas[:], ya_d[:, lg * TQ:(lg + 1) * TQ].rearrange("(c p) t -> p c t", p=128), writes=["yas"])
                P.dma("sp", ybs[:], yb_d[:, lg * TQ:(lg + 1) * TQ].rearrange("(c p) t -> p c t", p=128), writes=["ybs"])
                for oc in range(16):
                    pt, pk = pr.next()
                    mm_group(P, pt[:], [(wg[:, dc, oc * 128:(oc + 1) * 128], xqb[:, dc, :]) for dc in range(8)], ["wg", "xqb"], pk)
                    P.op("act", lambda e, d=sg[:, oc, :], p=pt: e.activation(out=d, in_=p[:], func=AF.Sigmoid), reads=[pk], writes=["sg"])
                for oc in range(8):
                    pa, pak = pr.next()
                    mm_group(P, pa[:], [(wa[:, fc, oc * 128:(oc + 1) * 128], yas[:, fc, :]) for fc in range(4)], ["wa", "yas"], pak)
                    pb, pbk = pr.next()
                    mm_group(P, pb[:], [(wb[:, fc, oc * 128:(oc + 1) * 128], ybs[:, fc, :]) for fc in range(4)], ["wb", "ybs"], pbk)
                    P.op("dve", lambda e, p=pa, g=sg[:, oc, :]: e.tensor_tensor(out=t1[:], in0=p[:], in1=g, op=ALU.mult), reads=[pak, "sg"], writes=["t1"])
                    P.op("dve", lambda e, p=pb, g=sg[:, 8 + oc, :]: e.tensor_tensor(out=t2[:], in0=p[:], in1=g, op=ALU.mult), reads=[pbk, "sg"], writes=["t2"])
                    P.op("pool", lambda e, d=mT[:, oc, :]: e.tensor_tensor(out=d, in0=t1[:], in1=t2[:], op=ALU.add), reads=["t1", "t2"], writes=["mT"])
                for tb in range(4):
                    blk = lg * 4 + tb
                    P.dma("sp", xres[:], xq[blk * 128:(blk + 1) * 128, :], writes=["xres"])
                    for half in range(2):
                        pm, pmk = pr.next()
                        mm_group(P, pm[:], [(mT[:, dc, tb * 128:(tb + 1) * 128], wo[:, dc, half * 512:(half + 1) * 512]) for dc in range(8)],
                                 ["mT", "wo"], pmk)
                        P.op("dve", lambda e, p=pm, hs=slice(half * 512, (half + 1) * 512): e.scalar_tensor_tensor(
                            out=r1[:, hs], in0=xres[:, hs], scalar=ALPHA, in1=p[:], op0=ALU.mult, op1=ALU.add),
                            reads=[pmk, "xres"], writes=["r1"])
                    layer_norm(P, r1, sq, sm, lnps, h1t, "r1", "h1t")
                    P.dma("sp", h1_d[blk * 128:(blk + 1) * 128, :], h1t[:], reads=["h1t"], writes=["h1_d%d" % blk])
                    for half in range(2):
                        ptt, ptk = pr.next()
                        mm_group(P, ptt[:], [(ptt[:, j * 128:(j + 1) * 128], h1t[:, (half * 4 + j) * 128:(half * 4 + j + 1) * 128], identf)
                                             for j in range(4)], ["h1t", "cf"], ptk)
                        P.op("act", lambda e, p=ptt, d=h1T[:, half * 4:(half + 1) * 4, :]: e.activation(
                            out=d.rearrange("p a b -> p (a b)"), in_=p[:], func=AF.Copy), reads=[ptk], writes=["h1T"])
                    prr, prk = pr.next()
                    mm_group(P, prr[:, 0:NE], [(h1T[:, dc, :], wr[:, dc, :]) for dc in range(8)], ["h1T", "wr"], prk)

                    lgt = sm[:, 16:48]
                    P.op("dve", lambda e, p=prr: e.tensor_tensor(out=sm[:, 16:48], in0=p[:, 0:NE], in1=brt[:], op=ALU.add),
                         reads=[prk, "brt"], writes=["rlg"])
                    P.op("dve", lambda e: e.max(out=sm[:, 0:8], in_=sm[:, 16:48]), reads=["rlg"], writes=["rtop"])
                    P.op("dve", lambda e, blk=blk: e.tensor_scalar(out=mask_f[:, blk, :], in0=sm[:, 16:48], scalar1=sm[:, 3:4], scalar2=None, op0=ALU.is_ge),
                         reads=["rlg", "rtop"], writes=["maskf"])
                    P.op("dve", lambda e, blk=blk: e.tensor_copy(out=mask_b[:, blk, :], in_=mask_f[:, blk, :]), reads=["maskf"], writes=["maskb"])
                    P.op("dve", lambda e: e.tensor_scalar(out=sm[:, 48:49], in0=sm[:, 0:1], scalar1=-1.0, scalar2=None, op0=ALU.mult),
                         reads=["rtop"], writes=["rneg"])
                    P.op("act", lambda e: e.activation(out=sm[:, 16:48], in_=sm[:, 16:48], func=AF.Exp, bias=sm[:, 48:49]),
                         reads=["rlg", "rneg", "maskf"], writes=["rex"])
                    P.op("dve", lambda e, blk=blk: e.tensor_tensor(out=sm[:, 16:48], in0=sm[:, 16:48], in1=mask_f[:, blk, :], op=ALU.mult),
                         reads=["rex", "maskf"], writes=["rexm"])
                    P.op("dve", lambda e: e.reduce_sum(out=sm[:, 8:9], in_=sm[:, 16:48], axis=mybir.AxisListType.X), reads=["rexm"], writes=["rden"])
                    P.op("dve", lambda e: e.reciprocal(out=sm[:, 9:10], in_=sm[:, 8:9]), reads=["rden"], writes=["rrd"])
                    P.op("dve", lambda e, blk=blk: e.tensor_scalar(out=gates_all[:, blk, :], in0=sm[:, 16:48], scalar1=sm[:, 9:10], scalar2=None, op0=ALU.mult),
                         reads=["rexm", "rrd"], writes=["gates", "rlg"])
            P.emit()

        with ExitStack() as stf:
          f = sb(stf, "f", [128, NBLK, D], F32)
          with ExitStack() as st:
            h1b = sb(st, "h1b", [128, NBLK, D], BF16)
            P.dma("pool", h1b[:], h1_d.rearrange("(b p) d -> p b d", p=128), reads=["h1_d"], writes=["h1b"])
            P.op("pool", lambda e: e.memset(f[:], 0.0), writes=["f%d" % b for b in range(NBLK)])
            wgur = Ring([sb(st, "wgu%d" % i, [128, 8, D], BF16) for i in range(2)], "wgu")
            wdnr = Ring([sb(st, "wdn%d" % i, [128, 4, D], BF16) for i in range(2)], "wdn")
            bgu = sb(st, "bgu", [128, NE * 16], F32)
            P.dma("sp", bgu[:], bGU[:, :], writes=["bgu"])
            posm = sb(st, "posm", [128, NBLK, NE], F32)
            Se = sb(st, "Se", [128, NBLK, CAP], BF16)
            STe = sb(st, "STe", [128, 3, NOWN], BF16)
            xg = sb(st, "xg", [128, 8, CAP], BF16)
            actT = sb(st, "actT", [128, 8, CAP], BF16)
            Ysb = sb(st, "Ysb", [128, 3, D], BF16)
            tar = Ring([sb(st, "ta%d" % i, [128, CAP], F32) for i in range(2)], "ta")
            tsr = Ring([sb(st, "tsg%d" % i, [128, CAP], F32) for i in range(2)], "tsg")
            tur = Ring([sb(st, "tu%d" % i, [128, CAP], F32) for i in range(2)], "tu")
            tgr = Ring([sb(st, "tg%d" % i, [128, CAP], F32) for i in range(2)], "tg")
            bgs = sb(st, "bgs", [128, NE * 16], F32)
            P.op("dve", lambda e: e.tensor_scalar(out=bgs[:], in0=bgu[:], scalar1=1.702, scalar2=None, op0=ALU.mult), reads=["bgu"], writes=["bgs"])
            pr = Ring(psb, "psb")
            for blk in range(NBLK):
                pt, pk = pr.next()
                pairs = [(ustrict, mask_b[:, blk, :])] + [(ones, mask_b[:, b2, :]) for b2 in range(blk)]
                mm_group(P, pt[:, 0:NE], pairs, ["cb", "maskb"], pk)

                P.op("dve", lambda e, p=pt, blk=blk: e.scalar_tensor_tensor(out=posm[:, blk, :], in0=p[:, 0:NE], scalar=1.0, in1=mask_f[:, blk, :],
                                                                           op0=ALU.add, op1=ALU.mult), reads=[pk, "maskf"], writes=["posm1"])
                P.op("dve", lambda e, blk=blk: e.tensor_scalar(out=posm[:, blk, :], in0=posm[:, blk, :], scalar1=-1.0, scalar2=None, op0=ALU.add),
                     reads=["posm1"], writes=["posm"])
            def build_Se(ex):
                for blk in range(NBLK):
                    P.op("dve", lambda e, d=Se[:, blk, :], s=posm[:, blk, ex:ex + 1]: e.tensor_scalar(
                        out=d, in0=iota, scalar1=s, scalar2=None, op0=ALU.is_equal), reads=["posm", "cf"], writes=["Se"])

            WG, WD = {}, {}

            def LOAD_GU(ex):
                WG[ex] = []
                for half in range(2):
                    t, k = wgur.next()
                    P.dma("pool", t[:], wGU[ex, :, half * D:(half + 1) * D].rearrange("(c p) n -> p c n", p=128), writes=[k])
                    WG[ex].append((t, k))

            def LOAD_DN(ex):
                WD[ex] = []
                for half in range(2):
                    t, k = wdnr.next()
                    P.dma("pool", t[:], wDN[ex, half * 512:(half + 1) * 512, :].rearrange("(c p) n -> p c n", p=128), writes=[k])
                    WD[ex].append((t, k))

            def ST(ex):
                for ci, (c0, cs) in enumerate(CBS):
                    for tq in range(4):
                        pt, pk = pr.next()
                        mm_group(P, pt[:], [(pt[0:cs, j * 128:(j + 1) * 128], Se[:, tq * 4 + j, c0:c0 + cs], ident) for j in range(4)],
                                 ["Se", "cb"], pk)
                        P.op("act", lambda e, p=pt, d=STe[0:cs, ci, tq * 512:(tq + 1) * 512], cs=cs: e.activation(out=d, in_=p[0:cs, :], func=AF.Copy),
                             reads=[pk], writes=["STe"])

            def GATHER(ex):
                for dc in range(8):
                    pt, pk = pr.next()
                    mm_group(P, pt[:, 0:CAP], [(h1b[:, blk, dc * 128:(dc + 1) * 128], Se[:, blk, :]) for blk in range(NBLK)],
                             ["h1b", "Se"], pk)
                    P.op("act", lambda e, p=pt, d=xg[:, dc, :]: e.activation(out=d, in_=p[:, 0:CAP], func=AF.Copy), reads=[pk], writes=["xg"])

            def GATEUP(ex):
                wa_, wak = WG[ex][0]
                wu_, wuk = WG[ex][1]
                for fc in range(8):
                    pa_, pak = pr.next()
                    mm_group(P, pa_[:, 0:CAP], [(wa_[:, dc, fc * 128:(fc + 1) * 128], xg[:, dc, :]) for dc in range(8)], [wak, "xg"], pak)
                    pu_, puk = pr.next()
                    mm_group(P, pu_[:, 0:CAP], [(wu_[:, dc, fc * 128:(fc + 1) * 128], xg[:, dc, :]) for dc in range(8)], [wuk, "xg"], puk)
                    ca = ex * 16 + fc
                    cu = ex * 16 + 8 + fc
                    ta, tak = tar.next()
                    tsg, tsk = tsr.next()
                    tu, tuk = tur.next()
                    tg, tgk = tgr.next()
                    P.op("dve", lambda e, p=pa_, b=bgu[:, ca:ca + 1], d=ta: e.tensor_scalar(out=d[:], in0=p[:, 0:CAP], scalar1=b, scalar2=7.0, op0=ALU.add, op1=ALU.min),
                         reads=[pak, "bgu"], writes=[tak])
                    P.op("act", lambda e, a=ta, d=tsg: e.activation(out=d[:], in_=a[:], func=AF.Sigmoid, scale=1.702),
                         reads=[tak], writes=[tsk])
                    P.op("dve", lambda e, p=pu_, b=bgu[:, cu:cu + 1], d=tu: e.tensor_scalar(out=d[:], in0=p[:, 0:CAP], scalar1=b, scalar2=7.0, op0=ALU.add, op1=ALU.min),
                         reads=[puk, "bgu"], writes=[tuk])
                    P.op("pool", lambda e, d=tg, a=ta, g=tsg: e.tensor_tensor(out=d[:], in0=a[:], in1=g[:], op=ALU.mult), reads=[tak, tsk], writes=[tgk])
                    P.op("dve", lambda e, d=tu: e.tensor_scalar(out=d[:], in0=d[:], scalar1=-7.0, scalar2=1.0, op0=ALU.max, op1=ALU.add),
                         reads=[tuk], writes=[tuk])
                    P.op("pool", lambda e, d=actT[:, fc, :], u=tu, g=tg: e.tensor_tensor(out=d, in0=u[:], in1=g[:], op=ALU.mult),
                         reads=[tuk, tgk], writes=["actT"])

            def DOWN(ex):
                wd_t = WD[ex]
                for ci, (c0, cs) in enumerate(CBS):
                    for half in range(2):
                        pt, pk = pr.next()
                        mm_group(P, pt[0:cs, :], [(actT[:, fc, c0:c0 + cs], wd_t[fc // 4][0][:, fc % 4, half * 512:(half + 1) * 512]) for fc in range(8)],
                                 ["actT", wd_t[0][1], wd_t[1][1]], pk)
                        P.op("act", lambda e, p=pt, d=Ysb[0:cs, ci, half * 512:(half + 1) * 512], cs=cs: e.activation(out=d, in_=p[0:cs, :], func=AF.Copy),
                             reads=[pk], writes=["Ysb"])

            def SCATTER(ex):
                for blk in range(NBLK):
                    for half in range(2):
                        pt, pk = pr.next()
                        mm_group(P, pt[:], [(STe[0:cs, ci, blk * 128:(blk + 1) * 128], Ysb[0:cs, ci, half * 512:(half + 1) * 512])
                                            for ci, (c0, cs) in enumerate(CBS)], ["STe", "Ysb"], pk)
                        fs = f[:, blk, half * 512:(half + 1) * 512]
                        P.op("dve", lambda e, p=pt, fs=fs, g=gates_all[:, blk, ex:ex + 1]: e.scalar_tensor_tensor(
                            out=fs, in0=p[:], scalar=g, in1=fs, op0=ALU.mult, op1=ALU.add),
                            reads=[pk, "gates", "f%d" % blk], writes=["f%d" % blk])

            LOAD_GU(0)
            LOAD_DN(0)
            build_Se(0)
            ST(0)
            GATHER(0)
            for ex in range(NE):
                if ex + 1 < NE:
                    build_Se(ex + 1)
                GATEUP(ex)
                if ex + 1 < NE:
                    LOAD_GU(ex + 1)
                    GATHER(ex + 1)
                DOWN(ex)
                if ex + 1 < NE:
                    LOAD_DN(ex + 1)
                SCATTER(ex)
                if ex + 1 < NE:
                    ST(ex + 1)
            if debug:
                P.dma("sp", dbg_f, f[:], reads=["f%d" % b for b in range(NBLK)], writes=["dbg_f"])
                P.dma("sp", dbg_g, gates_all[:], reads=["gates"], writes=["dbg_g"])
                P.dma("sp", dbg_p, posm[:], reads=["posm"], writes=["dbg_p"])
                P.dma("sp", dbg_x, xg[:], reads=["xg"], writes=["dbg_x"])
                P.dma("sp", dbg_a, actT[:], reads=["actT"], writes=["dbg_a"])
                P.dma("sp", dbg_y, Ysb[:], reads=["Ysb"], writes=["dbg_y"])
            P.emit()
          with ExitStack() as st:
            pr = Ring(psb, "psb")
            bdn = sb(st, "bdn", [NE, D], BF16)
            lnps = sb(st, "lnps2", [128, 2, D], F32)
            P.dma("pool", bdn[:], bDN[:, :], writes=["bdn"])
            P.dma("sp", lnps[:], lnp[2:4].rearrange("a p d -> p a d"), writes=["lnps"])
            h1r = Ring([sb(st, "h1r%d" % i, [128, D], F32) for i in range(2)], "h1r")
            r2s = [sb(st, "r2_%d" % i, [128, D], F32) for i in range(2)]
            sqs = [sb(st, "sq2_%d" % i, [128, D], F32) for i in range(2)]
            sms = [sb(st, "sm2_%d" % i, [128, 64], F32) for i in range(2)]
            gTs = [sb(st, "gT%d" % i, [NE, 128], BF16) for i in range(2)]
            outr = Ring([sb(st, "ot%d" % i, [128, D], F32) for i in range(2)], "ot")

            def final_block(Q, blk, ch):
                r2, sq, sm, gT = r2s[ch], sqs[ch], sms[ch], gTs[ch]
                sfx = "_%d" % ch
                ht, hk = h1r.next()
                Q.dma("sp", ht[:], h1_d[blk * 128:(blk + 1) * 128, :], reads=["h1_d"], writes=[hk])
                pt, pk = pr.next()
                mm_group(Q, pt[0:NE, 0:128], [(gates_all[:, blk, :], identf)], ["gates", "cf"], pk)
                Q.op("act", lambda e, p=pt: e.activation(out=gT[:], in_=p[0:NE, 0:128], func=AF.Copy), reads=[pk], writes=["gT" + sfx])
                for half in range(2):
                    hs = slice(half * 512, (half + 1) * 512)
                    pb_, pbk = pr.next()
                    mm_group(Q, pb_[:], [(gT[:], bdn[:, hs])], ["gT" + sfx, "bdn"], pbk)
                    Q.op("dve", lambda e, hs=hs, ht=ht, blk=blk: e.scalar_tensor_tensor(
                        out=r2[:, hs], in0=ht[:, hs], scalar=ALPHA, in1=f[:, blk, hs], op0=ALU.mult, op1=ALU.add),
                        reads=[hk, "f%d" % blk], writes=["r2" + sfx])
                    Q.op("dve", lambda e, hs=hs, p=pb_: e.tensor_tensor(out=r2[:, hs], in0=r2[:, hs], in1=p[:], op=ALU.add),
                         reads=[pbk, "r2" + sfx], writes=["r2" + sfx])
                ot, otk = outr.next()
                layer_norm(Q, r2, sq, sm, lnps, ot, "r2" + sfx, otk, sfx=sfx)
                Q.dma("sp", out[blk * 128:(blk + 1) * 128, :], ot[:], reads=[otk], writes=["out%d" % blk])

            for b0 in range(0, NBLK, 2):
                Qa, Qb = Deferred(P), Deferred(P)
                final_block(Qa, b0, 0)
                final_block(Qb, b0 + 1, 1)
                for fa, fb in zip(Qa.L, Qb.L):
                    fa()
                    fb()
            P.emit()
    return nc


class Deferred:
    def __init__(self, P):
        self.P = P
        self.L = []

    def op(self, *a, **k):
        self.L.append(lambda: self.P.op(*a, **k))

    def dma(self, *a, **k):
        self.L.append(lambda: self.P.dma(*a, **k))


def layer_norm(P, src, sq, sm, lnps, dst, skey, dkey, sfx=""):
    invn = 1.0 / D
    K = lambda n: n + sfx
    P.op("act", lambda e: e.activation(out=sq[:], in_=src[:], func=AF.Square), reads=[skey], writes=[K("sq")])
    P.op("dve", lambda e: e.tensor_scalar(out=dst[:], in0=src[:], scalar1=invn, scalar2=None, op0=ALU.mult, op1=ALU.add, accum_out=sm[:, 56:57]),
         reads=[skey], writes=[K("lmean"), dkey])
    P.op("dve", lambda e: e.tensor_scalar(out=dst[:], in0=sq[:], scalar1=invn, scalar2=None, op0=ALU.mult, op1=ALU.add, accum_out=sm[:, 57:58]),
         reads=[K("sq")], writes=[K("lex2"), dkey])
    P.op("dve", lambda e: e.tensor_tensor(out=sm[:, 58:59], in0=sm[:, 56:57], in1=sm[:, 56:57], op=ALU.mult), reads=[K("lmean")], writes=[K("lm2")])
    P.op("dve", lambda e: e.tensor_tensor(out=sm[:, 59:60], in0=sm[:, 57:58], in1=sm[:, 58:59], op=ALU.subtract), reads=[K("lex2"), K("lm2")], writes=[K("lvar0")])
    P.op("dve", lambda e: e.tensor_scalar(out=sm[:, 62:63], in0=sm[:, 59:60], scalar1=0.0, scalar2=LN_EPS, op0=ALU.max, op1=ALU.add),
         reads=[K("lvar0")], writes=[K("lvar")])
    P.op("act", lambda e: e.activation(out=sm[:, 60:61], in_=sm[:, 62:63], func=AF.Sqrt), reads=[K("lvar")], writes=[K("lstd")])
    P.op("dve", lambda e: e.reciprocal(out=sm[:, 61:62], in_=sm[:, 60:61]), reads=[K("lstd")], writes=[K("lrstd")])
    P.op("dve", lambda e: e.tensor_scalar(out=dst[:], in0=src[:], scalar1=sm[:, 56:57], scalar2=sm[:, 61:62], op0=ALU.subtract, op1=ALU.mult),
         reads=[K("lmean"), K("lrstd"), skey], writes=[dkey])
    P.op("dve", lambda e: e.tensor_tensor(out=dst[:], in0=dst[:], in1=lnps[:, 0, :], op=ALU.mult), reads=[dkey, "lnps"], writes=[dkey])
    P.op("dve", lambda e: e.tensor_tensor(out=dst[:], in0=dst[:], in1=lnps[:, 1, :], op=ALU.add), reads=[dkey, "lnps"], writes=[dkey])


def _t5_bucket(rel):
    n = np.maximum(rel, 0)
    nf = np.maximum(n, 1).astype(np.float32)
    large = 16 + (np.log(nf / np.float32(16)) / np.float32(math.log(128 / 16)) * np.float32(16)).astype(np.int32)
    large = np.minimum(large, 31)
    return np.where(n < 16, n, large)


def _host_tables(rel_bias, hf):
    ss = np.arange(128)[:, None]
    tt = np.arange(TQ)[None, :]
    offs = [0, 4] if hf == 0 else [4, 0]
    t5 = np.empty((2, 8, 9, 128, TQ), np.float32)
    sbm = np.empty((2, 8, 128, TQ), np.float32)
    cm = np.empty((2, 4, 128, 1024), np.float32)
    for par, off in enumerate(offs):
        for j in range(8):
            rel = 128 * (off - j) + tt - ss
            sbm[par, j] = np.where(rel > 0, 0.0, NEG)
        for jj in range(9):
            rel = 128 * (off - (jj - 1)) + tt - ss
            bk = _t5_bucket(rel)
            for h in range(8):
                t5[par, h, jj] = np.where(rel >= 0, rel_bias[bk, h], NEG)
        for tb in range(4):
            tl = np.arange(128)[:, None]
            sg = np.arange(1024)[None, :]
            rel = 128 * off + 128 * tb + tl - sg
            cm[par, tb] = np.where(rel >= 0, 0.0, -1e30)
    return t5.astype(bf16), sbm.astype(bf16), cm.astype(bf16)


_NC_CACHE = {}
_DEBUG = False


def kernel(x, w_in, w_branch_a, w_branch_b, w_out, rel_bias, ln1_g, ln1_b, w_router, b_router,
           w_gate_up, b_gate_up, w_down, b_down, ln2_g, ln2_b):
    x = np.asarray(x, np.float32)
    w = np.asarray(w_in, np.float32)[0]
    cs = np.cumsum([0, 512, 512, 512, 512, 64, 8, 512, 512, 512, 1024, 1024])
    qa, ka, va, qi, ki, wi, qb, kb, vb, ga, gb = [w[:, cs[i]:cs[i + 1]] for i in range(11)]
    shared = dict(
        wKA=np.ascontiguousarray(np.concatenate([ka, ki, ki, va], 1)),
        wKB=np.ascontiguousarray(np.concatenate([kb, vb], 1)),
        wQA=np.ascontiguousarray(np.concatenate([qa, qi], 1)),
        wWI=np.ascontiguousarray(wi),
        wQB=np.ascontiguousarray(qb),
        wG=np.ascontiguousarray(np.concatenate([ga, gb], 1)),
        wA=np.asarray(w_branch_a, np.float32)[0], wB=np.asarray(w_branch_b, np.float32)[0],
        wO=np.asarray(w_out, np.float32)[0], wR=np.asarray(w_router, np.float32)[0],
        wGU=np.asarray(w_gate_up, np.float32)[0], wDN=np.asarray(w_down, np.float32)[0],
        bGU=np.ascontiguousarray(np.asarray(b_gate_up, np.float32)[0].reshape(NE, 16, 128).transpose(2, 0, 1).reshape(128, NE * 16)),
        bDN=np.asarray(b_down, np.float32)[0],
        bRT=np.ascontiguousarray(np.broadcast_to(np.asarray(b_router, np.float32)[0][None, :], (128, NE))),
        lnp=np.ascontiguousarray(np.stack([np.broadcast_to(np.asarray(a, np.float32)[0][None, :], (128, D))
                                           for a in (ln1_g, ln1_b, ln2_g, ln2_b)])),
        rb31=np.ascontiguousarray(np.broadcast_to(np.asarray(rel_bias, np.float32)[31][None, :], (128, 8))),
    )
    eye = np.eye(128, dtype=np.float32)
    jj = np.arange(128)[:, None]
    ss = np.arange(128)[None, :]
    negU = np.where(jj >= ss, -1.0, 0.0)
    ustrict = np.where(jj < ss, 1.0, 0.0)
    shared["cst_b"] = np.concatenate([eye, negU, -np.ones((128, 128)), ustrict, np.ones((128, 128))], 1).astype(bf16)
    pow2 = np.broadcast_to((2.0 ** -np.arange(NIT + 1))[None, :], (128, NIT + 1))
    iota = np.broadcast_to(np.arange(CAP)[None, :], (128, CAP))
    shared["cst_f"] = np.ascontiguousarray(np.concatenate([eye, pow2, iota], 1).astype(np.float32))
    rb = np.asarray(rel_bias, np.float32)
    tabs = {hf: _host_tables(rb, hf) for hf in (0, 1)}
    in_maps = []
    for c in range(NCORES):
        b, hf = c // 2, c % 2
        cols = np.concatenate([np.arange(g * TQ, (g + 1) * TQ) for g in GROUPS[hf]])
        xb = x[b]
        m = dict(shared)
        m["xT"] = np.ascontiguousarray(xb.T)
        m["xTq"] = np.ascontiguousarray(xb[cols].T)
        m["xq"] = np.ascontiguousarray(xb[cols])
        m["t5tab"], m["sbmask"], m["cmask"] = tabs[hf]
        in_maps.append(m)
    if "nc" not in _NC_CACHE:
        _NC_CACHE["nc"] = build_program(debug=_DEBUG)
    res = run_bass_kernel_spmd(_NC_CACHE["nc"], in_maps, core_ids=list(range(NCORES)))
    if _DEBUG:
        _NC_CACHE["res"] = res
    outp = np.empty((4, S, D), np.float32)
    for c in range(NCORES):
        b, hf = c // 2, c % 2
        cols = np.concatenate([np.arange(g * TQ, (g + 1) * TQ) for g in GROUPS[hf]])
        outp[b, cols] = res.results[c]["out"]
    return outp
```
